# Optimizing a Trainium2 kernel written in Bass

```python
import math
import jax, jax.numpy as jnp
from jax import lax
import numpy as np


D_MODEL = 1024
BATCH = 4
SEQ = 4096
DEPTH = 4

HEAD_DIM = 64
N_HEADS_SB = 8
N_HEADS_FOX = 8
N_HEADS_MOBA = 8
BRANCH_WIDTH = 8 * HEAD_DIM
N_BRANCH = 3
D_FF = 4 * D_MODEL
Q_BLOCK = 128
MOBA_BLOCK = 256
MOBA_TOPK = 3
MOBA_Q_CHUNK = 32
RMS_EPS = 1e-6
NEG_INF = -1e30
FORGET_BIAS_INIT = 2.0
IN_COLS = 9 * BRANCH_WIDTH + N_HEADS_FOX + N_BRANCH * D_MODEL

kernel_name = "hybrid_stickbreak_fox_moba_gated"


def rmsnorm(x, g):
    xf = x.astype(jnp.float32)
    y = xf * lax.rsqrt(jnp.mean(xf * xf, axis=-1, keepdims=True) + RMS_EPS)
    return (y * g.astype(jnp.float32)).astype(x.dtype)


def split_in_proj(proj):
    sizes = [BRANCH_WIDTH] * 6 + [N_HEADS_FOX] + [BRANCH_WIDTH] * 3 + [N_BRANCH * D_MODEL]
    points = [int(p) for p in np.cumsum(sizes)[:-1]]
    return jnp.split(proj, points, axis=-1)


def to_heads(t, n_heads):
    B, S, _ = t.shape
    return t.reshape(B, S, n_heads, HEAD_DIM).transpose(0, 2, 1, 3)


def merge_heads(t):
    B, H, S, d = t.shape
    return t.transpose(0, 2, 1, 3).reshape(B, S, H * d)


def alibi_slopes(n):
    return 2.0 ** (-8.0 * jnp.arange(1, n + 1, dtype=jnp.float32) / n)


def stick_breaking_attention(q, k, v):
    B, H, S, d = q.shape
    scale = 1.0 / math.sqrt(d)
    kf = k.astype(jnp.float32)
    vf = v.astype(jnp.float32)
    key_pos = jnp.arange(S)

    def block(i):
        t0 = i * Q_BLOCK
        qb = lax.dynamic_slice_in_dim(q, t0, Q_BLOCK, axis=2).astype(jnp.float32)
        z = jnp.einsum('bhqd,bhkd->bhqk', qb, kf) * scale
        q_pos = t0 + jnp.arange(Q_BLOCK)
        past = key_pos[None, :] < q_pos[:, None]
        log_1m = jnp.where(past, -jax.nn.softplus(z), 0.0)
        tail = lax.cumsum(log_1m, axis=3, reverse=True) - log_1m
        w = jnp.where(past, jnp.exp(jax.nn.log_sigmoid(z) + tail), 0.0)
        return jnp.einsum('bhqk,bhkd->bhqd', w, vf)

    out = lax.map(block, jnp.arange(S // Q_BLOCK))
    out = out.transpose(1, 2, 0, 3, 4).reshape(B, H, S, d)
    return out.astype(q.dtype)


def forgetting_attention(q, k, v, f_logit):
    B, H, S, d = q.shape
    scale = 1.0 / math.sqrt(d)
    kf = k.astype(jnp.float32)
    vf = v.astype(jnp.float32)
    c = lax.cumsum(jax.nn.log_sigmoid(f_logit.astype(jnp.float32)), axis=2)
    key_pos = jnp.arange(S)

    def block(i):
        t0 = i * Q_BLOCK
        qb = lax.dynamic_slice_in_dim(q, t0, Q_BLOCK, axis=2).astype(jnp.float32)
        cq = lax.dynamic_slice_in_dim(c, t0, Q_BLOCK, axis=2)
        logits = jnp.einsum('bhqd,bhkd->bhqk', qb, kf) * scale + (cq[..., :, None] - c[..., None, :])
        q_pos = t0 + jnp.arange(Q_BLOCK)
        causal = key_pos[None, :] <= q_pos[:, None]
        p = jax.nn.softmax(jnp.where(causal, logits, NEG_INF), axis=-1)
        return jnp.einsum('bhqk,bhkd->bhqd', p, vf)

    out = lax.map(block, jnp.arange(S // Q_BLOCK))
    out = out.transpose(1, 2, 0, 3, 4).reshape(B, H, S, d)
    return out.astype(q.dtype)


def moba_attention(q, k, v, slopes):
    B, H, S, d = q.shape
    scale = 1.0 / math.sqrt(d)
    n_kb = -(-S // MOBA_BLOCK)
    n_top = min(MOBA_TOPK, n_kb)
    pad = n_kb * MOBA_BLOCK - S
    kf = jnp.pad(k.astype(jnp.float32), ((0, 0), (0, 0), (0, pad), (0, 0)))
    vf = jnp.pad(v.astype(jnp.float32), ((0, 0), (0, 0), (0, pad), (0, 0)))
    k_blocks = kf.reshape(B, H, n_kb, MOBA_BLOCK, d)
    v_blocks = vf.reshape(B, H, n_kb, MOBA_BLOCK, d)
    k_mean = jnp.mean(k_blocks, axis=3)
    b_idx = jnp.arange(B)[:, None, None, None]
    h_idx = jnp.arange(H)[None, :, None, None]
    blk_ids = jnp.arange(n_kb)
    offs = jnp.arange(MOBA_BLOCK)
    slope = slopes[None, :, None, None]
    n_sel = n_top * MOBA_BLOCK

    def chunk(c):
        t0 = c * MOBA_Q_CHUNK
        own = t0 // MOBA_BLOCK
        qc = lax.dynamic_slice_in_dim(q, t0, MOBA_Q_CHUNK, axis=2).astype(jnp.float32)
        q_pos = t0 + jnp.arange(MOBA_Q_CHUNK)
        route = jnp.einsum('bhqd,bhnd->bhqn', qc, k_mean)
        route = jnp.where(blk_ids < own, route, NEG_INF)
        _, sel = lax.top_k(route, n_top)
        sel_valid = sel < own
        k_sel = k_blocks[b_idx, h_idx, sel]
        v_sel = v_blocks[b_idx, h_idx, sel]
        s_sel = jnp.einsum('bhqd,bhqrnd->bhqrn', qc, k_sel) * scale
        pos_sel = sel[..., None] * MOBA_BLOCK + offs
        s_sel = s_sel - slope[..., None] * (q_pos[:, None, None] - pos_sel)
        s_sel = jnp.where(sel_valid[..., None], s_sel, NEG_INF).reshape(B, H, MOBA_Q_CHUNK, n_sel)
        k_own = lax.dynamic_slice_in_dim(kf, own * MOBA_BLOCK, MOBA_BLOCK, axis=2)
        v_own = lax.dynamic_slice_in_dim(vf, own * MOBA_BLOCK, MOBA_BLOCK, axis=2)
        rel = q_pos[:, None] - (own * MOBA_BLOCK + offs)[None, :]
        s_own = jnp.einsum('bhqd,bhnd->bhqn', qc, k_own) * scale - slope * rel
        s_own = jnp.where(rel >= 0, s_own, NEG_INF)
        p = jax.nn.softmax(jnp.concatenate([s_sel, s_own], axis=-1), axis=-1)
        p_sel = p[..., :n_sel].reshape(B, H, MOBA_Q_CHUNK, n_top, MOBA_BLOCK)
        p_own = p[..., n_sel:]
        return (jnp.einsum('bhqrn,bhqrnd->bhqd', p_sel, v_sel)
                + jnp.einsum('bhqn,bhnd->bhqd', p_own, v_own))

    out = lax.map(chunk, jnp.arange(S // MOBA_Q_CHUNK))
    out = out.transpose(1, 2, 0, 3, 4).reshape(B, H, S, d)
    return out.astype(q.dtype)


def setup_inputs(seed: int = 0) -> dict:
    key = jax.random.key(seed)
    ks = jax.random.split(key, 11)
    f32 = jnp.float32
    x = jax.random.normal(ks[0], (BATCH, SEQ, D_MODEL), f32)
    norm_mix = 1.0 + 0.02 * jax.random.normal(ks[1], (DEPTH, D_MODEL), f32)
    w_in = jax.random.normal(ks[2], (DEPTH, D_MODEL, IN_COLS), f32) * D_MODEL ** -0.5
    b_forget = FORGET_BIAS_INIT + 0.5 * jax.random.normal(ks[3], (DEPTH, N_HEADS_FOX), f32)
    w_branch = jax.random.normal(ks[4], (DEPTH, N_BRANCH, BRANCH_WIDTH, D_MODEL), f32) * BRANCH_WIDTH ** -0.5
    w_out = jax.random.normal(ks[5], (DEPTH, D_MODEL, D_MODEL), f32) * D_MODEL ** -0.5
    norm_mlp = 1.0 + 0.02 * jax.random.normal(ks[6], (DEPTH, D_MODEL), f32)
    w_up = jax.random.normal(ks[7], (DEPTH, D_MODEL, D_FF), f32) * D_MODEL ** -0.5
    w_down = jax.random.normal(ks[8], (DEPTH, D_FF, D_MODEL), f32) * D_FF ** -0.5
    norm_final = 1.0 + 0.02 * jax.random.normal(ks[9], (D_MODEL,), f32)
    return {"x": x, "norm_mix": norm_mix, "w_in": w_in, "b_forget": b_forget,
            "w_branch": w_branch, "w_out": w_out, "norm_mlp": norm_mlp,
            "w_up": w_up, "w_down": w_down, "norm_final": norm_final}


def reference(x, norm_mix, w_in, b_forget, w_branch, w_out, norm_mlp, w_up, w_down, norm_final):
    B, S, _ = x.shape
    slopes = alibi_slopes(N_HEADS_MOBA)
    for l in range(DEPTH):
        h = rmsnorm(x, norm_mix[l])
        proj = jnp.einsum('bsd,de->bse', h, w_in[l])
        q_a, k_a, v_a, q_b, k_b, v_b, f_b, q_c, k_c, v_c, g = split_in_proj(proj)
        o_a = stick_breaking_attention(to_heads(q_a, N_HEADS_SB), to_heads(k_a, N_HEADS_SB),
                                       to_heads(v_a, N_HEADS_SB))
        f_logit = (f_b + b_forget[l]).transpose(0, 2, 1)
        o_b = forgetting_attention(to_heads(q_b, N_HEADS_FOX), to_heads(k_b, N_HEADS_FOX),
                                   to_heads(v_b, N_HEADS_FOX), f_logit)
        o_c = moba_attention(to_heads(q_c, N_HEADS_MOBA), to_heads(k_c, N_HEADS_MOBA),
                             to_heads(v_c, N_HEADS_MOBA), slopes)
        branches = jnp.stack([merge_heads(o_a), merge_heads(o_b), merge_heads(o_c)], axis=2)
        lifted = jnp.einsum('bsnw,nwd->bsnd', branches, w_branch[l])
        gates = jax.nn.sigmoid(g.reshape(B, S, N_BRANCH, D_MODEL))
        mixed = jnp.sum(gates * lifted, axis=2)
        x = x + jnp.einsum('bsd,de->bse', mixed, w_out[l])
        h = rmsnorm(x, norm_mlp[l])
        hid = jnp.square(jax.nn.relu(jnp.einsum('bsd,df->bsf', h, w_up[l])))
        x = x + jnp.einsum('bsf,fd->bsd', hid, w_down[l])
    return rmsnorm(x, norm_final)
```

```python
import numpy as np
import ml_dtypes
import concourse.bass as bass
import concourse.mybir as mybir
from concourse.bass_utils import run_bass_kernel_spmd

F32 = mybir.dt.float32
BF16 = mybir.dt.bfloat16
AF = mybir.ActivationFunctionType
ALU = mybir.AluOpType
AX = mybir.AxisListType

D = 1024
HD = 64
NH = 8
BW = 512
DFF = 4096
IN_COLS = 9 * BW + NH + 3 * D
QKVF = 9 * BW + NH
OFF_Q = [0, 1536, 3080]
OFF_K = [512, 2048, 3592]
OFF_V = [1024, 2560, 4104]
OFF_F = 3072
OFF_G = 4616
RROWS = [64, 70, 84]
EPS = 1e-6
BIG = 30000.0
SBUF_BASE = 16640
SBUF_BYTES = 229000


class Buf:
    __slots__ = ("t", "w", "r", "name")

    def __init__(self, t, name=""):
        self.t = t
        self.w = {}
        self.r = {}
        self.name = name

    def __getitem__(self, idx):
        return self.t[idx]


class Eng:
    def __init__(self, name, sid):
        self.name = name
        self.sid = sid
        self.n = 0
        self.seen = {}
        self.prog = []


class Builder:
    def __init__(self, nc, n_dma_sp=12, n_dma_pool=2, n_dma_act=10):
        self.nc = nc
        self.sem_handles = []
        self.eng = {}
        self.dma_pools = {}
        self.dma_rr = {}
        self.sb_off = SBUF_BASE
        self.uid = 0
        self.arena = None
        self._n_sems = 5 + n_dma_sp + n_dma_pool + n_dma_act
        self._pool_sizes = {"sp": n_dma_sp, "pool": n_dma_pool, "act": n_dma_act}
        self.setup()

    def setup(self):
        i = 0
        for name in ["pe", "act", "dve", "pool", "sp"]:
            self.eng[name] = Eng(name, i)
            i += 1
        for q, n in self._pool_sizes.items():
            self.dma_pools[q] = [[i + j, 0] for j in range(n)]
            self.dma_rr[q] = 0
            i += n

    def sb(self, shape, dtype, name="t"):
        esz = 4 if dtype == F32 else 2
        nel = int(np.prod(shape[1:]))
        nbytes = nel * esz
        off = (self.sb_off + 31) // 32 * 32
        assert off + nbytes <= SBUF_BYTES, f"SBUF overflow {name} {off}+{nbytes}"
        if self.arena is None:
            self.arena = self.nc.alloc_sbuf_tensor_at("arena", [128, (SBUF_BYTES - SBUF_BASE) // 2], BF16, offset=SBUF_BASE)
        e0 = (off - SBUF_BASE) // 2
        v = self.arena[0:shape[0], e0:e0 + nbytes // 2]
        if dtype == F32:
            v = v.bitcast(F32)
        if len(shape) == 3:
            v = v.rearrange("p (a b) -> p a b", a=shape[1])
        self.sb_off = off + nbytes
        return Buf(v, name)

    def mark(self):
        return self.sb_off

    def release(self, mark):
        self.sb_off = mark

    def _need(self, reads, writes, accum):
        need = {}

        def merge(d):
            for k, v in d.items():
                if need.get(k, 0) < v:
                    need[k] = v

        for b in reads:
            merge(b.w)
        for b in writes:
            merge(b.w)
            merge(b.r)
        for b in accum:
            merge(b.r)
        return need

    def _emit_waits(self, e, need, own_ok=False):
        for sid, c in need.items():
            if sid == e.sid and not own_ok and e.name in ("pe", "sp"):
                continue
            if e.seen.get(sid, 0) < c:
                e.prog.append(("wait", sid, c))
                e.seen[sid] = c

    def op(self, en, fn, reads=(), writes=(), accum=(), sw=()):
        e = self.eng[en]
        self._emit_waits(e, self._need(reads, writes, accum))
        if sw:
            self._emit_waits(e, {k: v for b in sw for k, v in b.w.items() if k == e.sid}, own_ok=True)
        e.n += 1
        e.prog.append(("op", fn))
        for b in reads:
            b.r[e.sid] = e.n
        for b in writes:
            b.w = {e.sid: e.n}
            b.r = {}
        for b in accum:
            b.w[e.sid] = e.n

    def dma(self, qn, out_ap, in_ap, reads=(), writes=(), accum=()):
        e = self.eng[qn]
        pool = self.dma_pools[qn]
        s = pool[self.dma_rr[qn] % len(pool)]
        self.dma_rr[qn] += 1
        need = self._need(reads, writes, accum)
        if need.get(s[0], 0) < s[1]:
            need[s[0]] = s[1]
        self._emit_waits(e, need)
        s[1] += 16
        e.prog.append(("dma", out_ap, in_ap, s[0]))
        for b in reads:
            b.r[s[0]] = s[1]
        for b in writes:
            b.w = {s[0]: s[1]}
            b.r = {}
        for b in accum:
            b.w[s[0]] = s[1]

    def barrier(self):
        allc = {}
        for e in self.eng.values():
            allc[e.sid] = e.n
        for pool in self.dma_pools.values():
            for s in pool:
                allc[s[0]] = s[1]
        for e in self.eng.values():
            self._emit_waits(e, {k: v for k, v in allc.items() if v > 0})

    def emit(self, en, q):
        e = self.eng[en]
        sh = self.sem_handles
        mine = sh[e.sid]
        for it in e.prog:
            if it[0] == "wait":
                q.wait_ge(sh[it[1]], it[2])
            elif it[0] == "op":
                it[1](q).then_inc(mine, 1)
            else:
                q.dma_start(out=it[1], in_=it[2]).then_inc(sh[it[3]], 16)


class Rot:
    def __init__(self, bufs):
        self.bufs = bufs
        self.i = 0

    def next(self):
        b = self.bufs[self.i % len(self.bufs)]
        self.i += 1
        return b


def build_program(S, L, first_layer=0, n_layers_total=4, debug=False, stop_after=None):
    NT = S // 128
    NG = S // 512
    NB = 16
    nc = bass.Bass("TRN2", target_bir_lowering=False)
    dt = nc.dram_tensor
    x_in = dt("x", [S, D], F32, kind="ExternalInput").ap()
    norm_mix = dt("norm_mix", [L, 128, 8], F32, kind="ExternalInput").ap()
    w_in = dt("w_in", [L, D, IN_COLS], F32, kind="ExternalInput").ap()
    b_forget = dt("b_forget", [L, 8, 1], F32, kind="ExternalInput").ap()
    w_branch = dt("w_branch", [L, 3 * BW, D], F32, kind="ExternalInput").ap()
    w_out = dt("w_out", [L, D, D], F32, kind="ExternalInput").ap()
    norm_mlp = dt("norm_mlp", [L, 128, 8], F32, kind="ExternalInput").ap()
    w_up = dt("w_up", [L, D, DFF], F32, kind="ExternalInput").ap()
    w_down = dt("w_down", [L, DFF, D], F32, kind="ExternalInput").ap()
    norm_final = dt("norm_final", [128, D], F32, kind="ExternalInput").ap()
    c_ident = dt("c_ident", [128, 128], BF16, kind="ExternalInput").ap()
    c_ustrict = dt("c_ustrict", [128, 128], BF16, kind="ExternalInput").ap()
    c_nlt = dt("c_nlt", [128, 128], BF16, kind="ExternalInput").ap()
    c_maskaddlt = dt("c_maskaddlt", [128, 4, 512], F32, kind="ExternalInput").ap()
    c_maskadd = dt("c_maskadd", [128, 4, 512], F32, kind="ExternalInput").ap()
    c_pastadd = dt("c_pastadd", [128, NB, 128], F32, kind="ExternalInput").ap()
    c_validpast = dt("c_validpast", [128, NB, 128], BF16, kind="ExternalInput").ap()
    c_isown = dt("c_isown", [128, NB, 128], BF16, kind="ExternalInput").ap()
    c_foxq = dt("c_foxq", [8, 3, S], BF16, kind="ExternalInput").ap()
    c_mobq = dt("c_mobq", [8, 4, S], BF16, kind="ExternalInput").ap()
    c_mobk = dt("c_mobk", [8, 20, S], BF16, kind="ExternalInput").ap()
    y = dt("y", [S, D], F32, kind="ExternalOutput").ap()
    okind = "ExternalOutput" if debug else "Internal"
    xres = dt("xres", [S, D], F32, kind=okind).ap()
    QOP = [dt(f"qop{m}", [8, RROWS[m], S], BF16, kind=okind).ap() for m in range(3)]
    KOP = [dt(f"kop{m}", [8, RROWS[m], S], BF16, kind=okind).ap() for m in range(3)]
    VV = [dt(f"vv{m}", [S, BW], BF16, kind=okind).ap() for m in range(3)]
    OT = dt("ot", [3 * BW, S], BF16, kind=okind).ap()

    B = Builder(nc)
    op, dma = B.op, B.dma

    psf = [Buf(nc.alloc_psum_tensor(f"psf{i}", [128, 512], F32), f"psf{i}") for i in range(6)]
    psb = [Buf(nc.alloc_psum_tensor(f"psb{i}", [128, 1024], BF16), f"psb{i}") for i in range(2)]

    ident = B.sb([128, 128], BF16, "ident")
    nU = B.sb([128, 128], BF16, "nU")
    nLT = B.sb([128, 128], BF16, "nLT")
    maskadd = B.sb([128, 4, 512], F32, "maskadd")
    maskaddlt = B.sb([128, 4, 512], F32, "maskaddlt")
    KM = B.sb([128, 4, NB], F32, "KM")
    KMb = B.sb([128, 8, NB], BF16, "KMb")
    Gt = B.sb([128, 8], F32, "Gt")
    G2t = B.sb([128, 8], F32, "G2t")
    nbf = B.sb([8, 1], F32, "nbf")
    persist_mark = B.mark()

    dma("sp", ident[:], c_ident, writes=[ident])
    dma("sp", nU[:], c_ustrict, writes=[nU])
    dma("sp", nLT[:], c_nlt, writes=[nLT])
    dma("sp", maskadd[:], c_maskadd, writes=[maskadd])
    dma("sp", maskaddlt[:], c_maskaddlt, writes=[maskaddlt])
    op("pool", lambda q: q.memset(KM[:], 0.0), writes=[KM])
    op("pool", lambda q: q.memset(KMb[:], 0.0), writes=[KMb])
    dma("sp", QOP[1][:, 67:70, :], c_foxq)
    dma("sp", KOP[1][:, 64:67, :], c_foxq)
    dma("sp", QOP[2][:, 80:84, :], c_mobq)
    dma("sp", KOP[2][:, 64:84, :], c_mobk)
    B.barrier()

    cast_rr = [0]

    def cast_scaled(out_ap, in_ap, scal_ap, reads, accum):
        k = (0, 2)[cast_rr[0] % 2]
        cast_rr[0] += 1
        if scal_ap is None:
            if k == 0:
                op("dve", lambda q: q.tensor_copy(out=out_ap, in_=in_ap), reads=reads, accum=accum)
            elif k == 1:
                op("pool", lambda q: q.tensor_copy(out=out_ap, in_=in_ap), reads=reads, accum=accum)
            else:
                op("act", lambda q: q.copy(out=out_ap, in_=in_ap), reads=reads, accum=accum)
        else:
            if k == 0:
                op("dve", lambda q: q.tensor_scalar(out=out_ap, in0=in_ap, scalar1=scal_ap, scalar2=None, op0=ALU.mult), reads=reads, accum=accum)
            elif k == 1:
                op("pool", lambda q: q.tensor_scalar(out=out_ap, in0=in_ap, scalar1=scal_ap, scalar2=None, op0=ALU.mult), reads=reads, accum=accum)
            else:
                op("act", lambda q: q.mul(out=out_ap, in_=in_ap, mul=scal_ap), reads=reads, accum=accum)

    def load_weight(Wb, nchunk, ncols, src_fn, gt, stages, cw=1024):
        for c in range(nchunk):
            for c0 in range(0, ncols, cw):
                c1 = min(ncols, c0 + cw)
                st = stages.next()
                dma("sp", st[:, 0:c1 - c0], src_fn(c, c0, c1), writes=[st])
                sc = None if gt is None else gt[:, c:c + 1]
                rd = [st] if gt is None else [st, gt]
                cast_scaled(Wb[:, c, c0:c1], st[:, 0:c1 - c0], sc, rd, [Wb])

    def norm_transpose(xt, hT_ap_fn, hT_buf, ssq_rot, hb_rot, tp, evac_eng):
        ssq = ssq_rot.next()
        hb = hb_rot.next()
        op("act", lambda q: q.activation(out=hb[:], in_=xt_ap(xt), func=AF.Square, accum_out=ssq[:, 0:1]),
           reads=[xt[0]], writes=[hb, ssq])
        op("act", lambda q: q.activation(out=ssq[:, 1:2], in_=ssq[:, 0:1], func=AF.Sqrt, scale=1.0 / D, bias=epsb[:, 0:1]),
           reads=[ssq, epsb], accum=[ssq])
        op("dve", lambda q: q.reciprocal(out=ssq[:, 2:3], in_=ssq[:, 1:2]), reads=[ssq], accum=[ssq])
        op("act", lambda q: q.activation(out=hb[:], in_=xt_ap(xt), func=AF.Copy, scale=ssq[:, 2:3]),
           reads=[xt[0], ssq], writes=[hb])
        for kc in range(8):
            op("pe", lambda q, kc=kc: q.transpose(out=tp[:, kc * 128:(kc + 1) * 128], in_=hb[:, kc * 128:(kc + 1) * 128], identity=ident[:]),
               reads=[hb, ident], writes=[tp] if kc == 0 else (), accum=() if kc == 0 else [tp])
        src = tp[:, :].rearrange("p (k t) -> p k t", k=8)
        if evac_eng == "dve":
            op("dve", lambda q: q.tensor_copy(out=hT_ap_fn(), in_=src), reads=[tp], accum=[hT_buf])
        else:
            op("act", lambda q: q.copy(out=hT_ap_fn(), in_=src), reads=[tp], accum=[hT_buf])
        return ssq

    def xt_ap(xt):
        return xt[1]

    epsb = B.sb([128, 1], F32, "epsb")
    op("pool", lambda q: q.memset(epsb[:], EPS), writes=[epsb])
    persist_mark = B.mark()

    for li in range(L):
        if stop_after == "init":
            break
        lg = first_layer + li
        xsrc = x_in if li == 0 else xres
        last = (lg == n_layers_total - 1)
        B.release(persist_mark)
        dma("sp", Gt[:], norm_mix[li], writes=[Gt])
        dma("sp", G2t[:], norm_mlp[li], writes=[G2t])
        dma("sp", nbf[:], b_forget[li], writes=[nbf])
        op("dve", lambda q: q.tensor_scalar(out=nbf[:], in0=nbf[:], scalar1=-1.0, scalar2=None, op0=ALU.mult), reads=[nbf], writes=[nbf])
        Wb = B.sb([128, 8, QKVF], BF16, "Wb")
        stages = Rot([B.sb([128, 1024], F32, "stg") for _ in range(6)])
        load_weight(Wb, 8, QKVF, lambda c, c0, c1: w_in[li, c * 128:(c + 1) * 128, c0:c1], Gt, stages)
        SA = {"A0": 0, "A1": 1, "A2": 2, "A3": 3, "A4": 4, "A42": 4.2, "A44": 4.4, "A46": 4.6, "A48": 4.8}.get(stop_after, 9)
        xrot = Rot([B.sb([128, D], F32, "xt") for _ in range(3)])
        hbrot = Rot([B.sb([128, D], BF16, "hb") for _ in range(2)])
        ssqrot = Rot([B.sb([128, 4], F32, "ssq") for _ in range(4)])
        hTrot = Rot([B.sb([128, 8, 512], BF16, "hT") for _ in range(3)])
        evrot = Rot([B.sb([128, 512], BF16, "ev") for _ in range(4)])
        Qc = B.sb([128, 4, 512], BF16, "Qc")
        onesf = B.sb([8, 512], F32, "onesf")
        op("pool", lambda q: q.memset(onesf[:], 1.0), writes=[onesf])
        fe = B.sb([8, 512], F32, "fe")
        fsp = B.sb([8, 512], F32, "fsp")
        Crot = Rot([B.sb([8, 512], F32, "Cg") for _ in range(2)])
        r1 = B.sb([8, 512], F32, "r1")
        r2 = B.sb([8, 512], F32, "r2")
        CQt = B.sb([8, 3, 512], BF16, "CQt")
        CKt = B.sb([8, 3, 512], BF16, "CKt")
        pastadd = B.sb([128, NB, 128], F32, "pastadd")
        validpast = B.sb([128, NB, 128], BF16, "validpast")
        isown = B.sb([128, NB, 128], BF16, "isown")
        dma("sp", pastadd[:], c_pastadd, writes=[pastadd])
        dma("sp", validpast[:], c_validpast, writes=[validpast])
        dma("sp", isown[:], c_isown, writes=[isown])
        rm = B.sb([128, 128], F32, "rm")
        mx8 = B.sb([128, 8, 8], F32, "mx8")
        sel = B.sb([128, 128], F32, "sel")
        mbt = B.sb([128, 128], BF16, "mbt")
        MBT = B.sb([128, 512], BF16, "MBT")
        czero = B.sb([8, 1], F32, "czero")
        op("pool", lambda q: q.memset(czero[:], 0.0), writes=[czero])
        cprev = czero
        psrot = Rot(psf[0:4])
        ev_i = [0]

        def evac(out_ap, in_ap, scale, reads, writes):
            k = ev_i[0] % 2
            ev_i[0] += 1
            if k == 0:
                if scale is None:
                    op("dve", lambda q: q.tensor_copy(out=out_ap, in_=in_ap), reads=reads, writes=writes)
                else:
                    op("dve", lambda q: q.tensor_scalar(out=out_ap, in0=in_ap, scalar1=scale, scalar2=None, op0=ALU.mult), reads=reads, writes=writes)
            else:
                if scale is None:
                    op("act", lambda q: q.copy(out=out_ap, in_=in_ap), reads=reads, writes=writes)
                else:
                    op("act", lambda q: q.mul(out=out_ap, in_=in_ap, mul=scale), reads=reads, writes=writes)

        hTs = {}
        cstate = {'cprev': cprev}

        def prepA(g):
            tok0 = g * 512
            hT = hTrot.next()
            for t in range(4):
                xt = xrot.next()
                dma("sp", xt[:], xsrc[tok0 + t * 128: tok0 + (t + 1) * 128, :], writes=[xt])
                norm_transpose((xt, xt[:]), lambda hT=hT, t=t: hT[:, :, t * 128:(t + 1) * 128], hT, ssqrot, hbrot, psb[0],
                               "dve" if t % 2 == 0 else "act")
            hTs[g] = hT

        def proj_qk(g, ms):
            tok0 = g * 512
            hT = hTs[g]
            for m in ms:
                for which in range(2):
                    base = (OFF_Q if which == 0 else OFF_K)[m]
                    dst = (QOP if which == 0 else KOP)[m]
                    for cc in range(4):
                        ps = psrot.next()
                        col0 = base + cc * 128
                        for kc in range(8):
                            op("pe", lambda q, ps=ps, kc=kc, col0=col0, hT=hT: q.matmul(ps[:], lhsT=Wb[:, kc, col0:col0 + 128], rhs=hT[:, kc, :], start=(kc == 0), stop=(kc == 7)),
                               reads=[Wb, hT], writes=[ps] if kc == 0 else (), accum=() if kc == 0 else [ps])
                        if m == 2 and which == 0:
                            op("dve", lambda q, ps=ps, cc=cc: q.tensor_scalar(out=Qc[:, cc, :], in0=ps[:], scalar1=0.125, scalar2=None, op0=ALU.mult),
                               reads=[ps], accum=[Qc])
                            for hh in range(2):
                                dma("act", dst[cc * 2 + hh, 0:64, tok0:tok0 + 512], Qc[hh * 64:(hh + 1) * 64, cc, :], reads=[Qc])
                            continue
                        ev = evrot.next()
                        evac(ev[:], ps[:], 0.125 if which == 0 else None, [ps], [ev])
                        if m == 2 and which == 1:
                            op("dve", lambda q, ps=ps, cc=cc, g=g: q.tensor_reduce(out=KM[:, cc, 2 * g:2 * g + 2], in_=ps[:, :].rearrange("p (b t) -> p b t", b=2), axis=AX.X, op=ALU.add),
                               reads=[ps], accum=[KM])
                            for hh in range(2):
                                op("dve", lambda q, cc=cc, g=g, hh=hh: q.tensor_copy(out=KMb[hh * 64:(hh + 1) * 64, cc * 2 + hh, 2 * g:2 * g + 2], in_=KM[hh * 64:(hh + 1) * 64, cc, 2 * g:2 * g + 2]),
                                   reads=[KM], accum=[KMb])
                        for hh in range(2):
                            dma("act", dst[cc * 2 + hh, 0:64, tok0:tok0 + 512], ev[hh * 64:(hh + 1) * 64, :], reads=[ev])

        def proj_v(g):
            tok0 = g * 512
            hT = hTs[g]
            for m in range(3):
                for t in range(4):
                    ps = psrot.next()
                    for kc in range(8):
                        op("pe", lambda q, ps=ps, kc=kc, m=m, t=t, hT=hT: q.matmul(ps[:], lhsT=hT[:, kc, t * 128:(t + 1) * 128], rhs=Wb[:, kc, OFF_V[m]:OFF_V[m] + 512], start=(kc == 0), stop=(kc == 7)),
                           reads=[Wb, hT], writes=[ps] if kc == 0 else (), accum=() if kc == 0 else [ps])
                    ev = evrot.next()
                    evac(ev[:], ps[:], None, [ps], [ev])
                    dma("act", VV[m][tok0 + t * 128: tok0 + (t + 1) * 128, :], ev[:], reads=[ev])

        def tailA(g):
            tok0 = g * 512
            hT = hTs[g]
            cprev = cstate['cprev']
            ps = psrot.next()
            for kc in range(8):
                op("pe", lambda q, ps=ps, kc=kc, hT=hT: q.matmul(ps[0:8, :], lhsT=Wb[:, kc, OFF_F:OFF_F + 8], rhs=hT[:, kc, :], start=(kc == 0), stop=(kc == 7)),
                   reads=[Wb, hT], writes=[ps] if kc == 0 else (), accum=() if kc == 0 else [ps])
            op("act", lambda q, ps=ps: q.activation(out=fe[:], in_=ps[0:8, :], func=AF.Exp, scale=-1.0, bias=nbf[:, 0:1]), reads=[ps, nbf], writes=[fe])
            op("act", lambda q: q.activation(out=fsp[:], in_=fe[:], func=AF.Ln, bias=1.0), reads=[fe], writes=[fsp])
            Cg = Crot.next()
            init_ap = cprev[:, 0:1] if cprev is czero else cprev[:, 511:512]
            op("dve", lambda q, Cg=Cg, init_ap=init_ap, onesf=onesf, fsp=fsp: q.tensor_tensor_scan(out=Cg[:], data0=onesf[:], data1=fsp[:], initial=init_ap, op0=ALU.mult, op1=ALU.subtract),
               reads=[onesf, fsp, cprev], writes=[Cg], sw=[cprev])
            cstate['cprev'] = Cg
            cprev = Cg
            op("dve", lambda q, Cg=Cg: q.tensor_copy(out=CQt[:, 0, :], in_=Cg[:]), reads=[Cg], writes=[CQt])
            op("dve", lambda q, Cg=Cg: q.tensor_tensor(out=r1[:], in0=Cg[:], in1=CQt[:, 0, :], op=ALU.subtract), reads=[Cg, CQt], writes=[r1])
            op("dve", lambda q: q.tensor_copy(out=CQt[:, 1, :], in_=r1[:]), reads=[r1], accum=[CQt])
            op("dve", lambda q: q.tensor_tensor(out=r2[:], in0=r1[:], in1=CQt[:, 1, :], op=ALU.subtract), reads=[r1, CQt], writes=[r2])
            op("dve", lambda q: q.tensor_copy(out=CQt[:, 2, :], in_=r2[:]), reads=[r2], accum=[CQt])
            op("dve", lambda q: q.tensor_scalar(out=CKt[:], in0=CQt[:], scalar1=-1.0, scalar2=None, op0=ALU.mult), reads=[CQt], writes=[CKt])
            dma("act", QOP[1][:, 64:67, tok0:tok0 + 512], CQt[:], reads=[CQt])
            dma("act", KOP[1][:, 67:70, tok0:tok0 + 512], CKt[:], reads=[CKt])
            for t in range(4):
                own = (tok0 + t * 128) // 256
                ps = psrot.next()
                for h in range(8):
                    pb = (h % 2) * 64
                    op("pe", lambda q, ps=ps, h=h, pb=pb, t=t: q.matmul(ps[:, h * 16:(h + 1) * 16], lhsT=Qc[:, h // 2, t * 128:(t + 1) * 128], rhs=KMb[:, h, :], start=True, stop=True),
                       reads=[Qc, KMb], writes=[ps] if h == 0 else (), accum=() if h == 0 else [ps])
                op("dve", lambda q, ps=ps, own=own: q.tensor_tensor(out=rm[:], in0=ps[:, 0:128], in1=pastadd[:, own, :], op=ALU.add), reads=[ps, pastadd], writes=[rm])
                for h in range(8):
                    op("dve", lambda q, h=h: q.max(out=mx8[:, h, :], in_=rm[:, h * 16:(h + 1) * 16]), reads=[rm], writes=[mx8] if h == 0 else (), accum=() if h == 0 else [mx8])
                for h in range(8):
                    op("dve", lambda q, h=h: q.tensor_scalar(out=sel[:, h * 16:(h + 1) * 16], in0=rm[:, h * 16:(h + 1) * 16], scalar1=mx8[:, h, 2:3], scalar2=None, op0=ALU.is_ge),
                       reads=[rm, mx8], writes=[sel] if h == 0 else (), accum=() if h == 0 else [sel], sw=[mx8])
                op("dve", lambda q, own=own: q.tensor_tensor(out=sel[:], in0=sel[:], in1=validpast[:, own, :], op=ALU.mult), reads=[validpast], writes=[sel])
                op("dve", lambda q, own=own: q.tensor_tensor(out=sel[:], in0=sel[:], in1=isown[:, own, :], op=ALU.add), reads=[isown], writes=[sel])
                op("dve", lambda q: q.tensor_scalar(out=mbt[:], in0=sel[:], scalar1=BIG, scalar2=-BIG, op0=ALU.mult, op1=ALU.add), reads=[sel], writes=[mbt])
                tp = psb[1]
                op("pe", lambda q, tp=tp: q.transpose(out=tp[:, 0:128], in_=mbt[:], identity=ident[:]), reads=[mbt, ident], writes=[tp])
                op("act", lambda q, tp=tp, t=t: q.copy(out=MBT[:, t * 128:(t + 1) * 128], in_=tp[:, 0:128]), reads=[tp], accum=[MBT])
            for h in range(8):
                dma("act", QOP[2][h, 64:80, tok0:tok0 + 512], MBT[h * 16:(h + 1) * 16, :], reads=[MBT])

        prepA(0)
        for g in range(NG):
            proj_qk(g, [0])
            if g + 1 < NG:
                prepA(g + 1)
            proj_qk(g, [1])
            if g > 0:
                tailA(g - 1)
            proj_qk(g, [2])
            proj_v(g)
        tailA(NG - 1)
        B.barrier()
        if stop_after is not None and stop_after.startswith("A"):
            break

        B.release(persist_mark)
        Vall = B.sb([128, NT, 512], BF16, "Vall")
        VHrot = Rot([B.sb([128, NT, 128], BF16, "VH") for _ in range(4)])
        for vb in VHrot.bufs:
            op("pool", lambda q, vb=vb: q.memset(vb[:], 1.0), writes=[vb])
        Koprot = Rot([B.sb([84, S], BF16, "Kop") for _ in range(4)])

        class Slot:
            pass

        slots = []
        for si in range(2):
            sl = Slot()
            sl.Qop = Rot([B.sb([84, 512], BF16, "Qop") for _ in range(2)])
            sl.Zm = Rot([B.sb([128, 512], F32, "Zm") for _ in range(2)])
            sl.E = Rot([B.sb([128, 512], F32, "E") for _ in range(2)])
            sl.SPt = Rot([B.sb([128, 512], F32, "SPt") for _ in range(2)])
            sl.SPh = Rot([B.sb([128, 512], BF16, "SPh") for _ in range(4)])
            sl.SPl = Rot([B.sb([128, 512], BF16, "SPl") for _ in range(3)])
            sl.A = Rot([B.sb([128, 512], F32, "A") for _ in range(3)])
            sl.W = Rot([B.sb([128, 512], BF16, "W") for _ in range(3)])
            sl.rc = B.sb([128, 512], F32, "rc")
            sl.ON = Rot([B.sb([64, 512], BF16, "ON") for _ in range(2)])
            sl.Zr = Rot([psf[si], Buf(psb[si].t[:, :].bitcast(F32), f"psbf{si}")])
            sl.ACC = psf[2 + si]
            sl.O = psf[4 + si]
            sl.Orot = Rot([psf[4 + si], psf[2 + si]])
            slots.append(sl)

        def run_jobs(m, jobs):
            R = RROWS[m]
            g = jobs[0][2]
            nk = 4 * (g + 1)
            st = []
            for (sl, h, g_, Kop, VH) in jobs:
                Qop = sl.Qop.next()
                dma("sp", Qop[0:R, :], QOP[m][h, :, g * 512:(g + 1) * 512], writes=[Qop])
                st.append({"Qop": Qop, "tiles": {}, "O": sl.O if m == 0 else sl.Orot.next()})

            def kc_of(i):
                return nk - 1 - i if m == 0 else i

            def stage_s(j, i):
                sl, h, g_, Kop, VH = jobs[j]
                Qop = st[j]["Qop"]
                kc = kc_of(i)
                v = kc - 4 * g
                T = {}
                st[j]["tiles"][i] = T
                Z = sl.Zr.next()
                op("pe", lambda q, Z=Z, Kop=Kop, Qop=Qop, kc=kc: q.matmul(Z[:], lhsT=Kop[0:R, kc * 128:(kc + 1) * 128], rhs=Qop[0:R, :], start=True, stop=True),
                   reads=[Kop, Qop], writes=[Z])
                T["zsrc"], T["zb"] = Z[:], Z
                if v >= 0:
                    Zm = sl.Zm.next()
                    madd = maskaddlt if m == 0 else maskadd
                    op("dve", lambda q, Zm=Zm, Z=Z, v=v, madd=madd: q.tensor_tensor(out=Zm[:], in0=Z[:], in1=madd[:, v, :], op=ALU.add), reads=[Z, madd], writes=[Zm])
                    T["zsrc"], T["zb"] = Zm[:], Zm

            def stage_act(j, i, sub=0):
                sl, h, g_, Kop, VH = jobs[j]
                T = st[j]["tiles"][i]
                zsrc, zb = T["zsrc"], T["zb"]
                if m > 0:
                    W = sl.W.next()
                    op("act", lambda q, W=W, zsrc=zsrc: q.activation(out=W[:], in_=zsrc, func=AF.Exp), reads=[zb], writes=[W])
                    T["W"] = W
                    return
                if sub == 0:
                    E = sl.E.next()
                    op("act", lambda q, E=E, zsrc=zsrc: q.activation(out=E[:], in_=zsrc, func=AF.Exp), reads=[zb], writes=[E])
                    T["E"] = E
                elif sub == 1:
                    E = T["E"]
                    SPt = sl.SPt.next()
                    op("act", lambda q, E=E, SPt=SPt: q.activation(out=SPt[:], in_=E[:], func=AF.Ln, bias=1.0), reads=[E], writes=[SPt])
                    T["SPt"] = SPt
                else:
                    SPt = T["SPt"]
                    SPh = sl.SPh.next()
                    op("act", lambda q, SPt=SPt, SPh=SPh: q.copy(out=SPh[:], in_=SPt[:]), reads=[SPt], writes=[SPh])
                    T["SPh"] = SPh

            def stage_dve1(j, i):
                sl, h, g_, Kop, VH = jobs[j]
                T = st[j]["tiles"][i]
                zsrc, zb, SPt, SPh = T["zsrc"], T["zb"], T["SPt"], T["SPh"]
                SPl = sl.SPl.next()
                A = sl.A.next()
                op("dve", lambda q, SPt=SPt, SPh=SPh, SPl=SPl: q.tensor_tensor(out=SPl[:], in0=SPt[:], in1=SPh[:], op=ALU.subtract), reads=[SPt, SPh], writes=[SPl])
                op("dve", lambda q, A=A, zsrc=zsrc, SPt=SPt: q.tensor_tensor(out=A[:], in0=zsrc, in1=SPt[:], op=ALU.subtract), reads=[zb, SPt], writes=[A])
                T.update(SPl=SPl, A=A)

            def stage_u(j, i):
                sl, h, g_, Kop, VH = jobs[j]
                T = st[j]["tiles"][i]
                ACC = sl.ACC
                SPh, SPl = T["SPh"], T["SPl"]
                lastc = (i == nk - 1)
                op("pe", lambda q, ACC=ACC, SPh=SPh, i=i: q.matmul(ACC[:], lhsT=nU[:], rhs=SPh[:], start=(i == 0), stop=False), reads=[nU, SPh],
                   writes=[ACC] if i == 0 else (), accum=() if i == 0 else [ACC])
                op("pe", lambda q, ACC=ACC, SPl=SPl, lastc=lastc: q.matmul(ACC[:], lhsT=nU[:], rhs=SPl[:], start=False, stop=lastc), reads=[nU, SPl], accum=[ACC])

            def stage_a2(j, i):
                sl, h, g_, Kop, VH = jobs[j]
                T = st[j]["tiles"][i]
                ACC = sl.ACC
                A = T["A"]
                op("dve", lambda q, A=A, ACC=ACC: q.tensor_tensor(out=A[:], in0=ACC[:], in1=A[:], op=ALU.add), reads=[ACC], writes=[A])

            def stage_w(j, i):
                sl, h, g_, Kop, VH = jobs[j]
                T = st[j]["tiles"][i]
                A = T["A"]
                W = sl.W.next()
                op("act", lambda q, A=A, W=W: q.activation(out=W[:], in_=A[:], func=AF.Exp), reads=[A], writes=[W])
                T["W"] = W

            def stage_lt(j, i):
                sl, h, g_, Kop, VH = jobs[j]
                if i == nk - 1:
                    return
                T = st[j]["tiles"][i]
                ACC = sl.ACC
                SPh, SPl = T["SPh"], T["SPl"]
                op("pe", lambda q, ACC=ACC, SPh=SPh: q.matmul(ACC[:], lhsT=nLT[:], rhs=SPh[:], start=False, stop=False), reads=[nLT, SPh], accum=[ACC])
                op("pe", lambda q, ACC=ACC, SPl=SPl: q.matmul(ACC[:], lhsT=nLT[:], rhs=SPl[:], start=False, stop=False), reads=[nLT, SPl], accum=[ACC])

            def stage_pv(j, i):
                sl, h, g_, Kop, VH = jobs[j]
                T = st[j]["tiles"].pop(i)
                W = T["W"]
                kc = kc_of(i)
                O = st[j]["O"]
                if m == 0:
                    op("pe", lambda q, O=O, W=W, kc=kc, h=h, i=i: q.matmul(O[0:64, :], lhsT=Vall[:, kc, h * 64:(h + 1) * 64], rhs=W[:], start=(i == 0), stop=(i == nk - 1)),
                       reads=[Vall, W], writes=[O] if i == 0 else (), accum=() if i == 0 else [O])
                else:
                    op("pe", lambda q, O=O, W=W, kc=kc, VH=VH, i=i: q.matmul(O[:], lhsT=VH[:, kc, :], rhs=W[:], start=(i == 0), stop=(i == nk - 1)),
                       reads=[VH, W], writes=[O] if i == 0 else (), accum=() if i == 0 else [O])

            nj = len(jobs)
            J = range(nj)
            if m == 0:
                for t in range(nk + 3):
                    if t < nk:
                        for j in J:
                            stage_s(j, t)
                        for sub in range(3):
                            for j in J:
                                stage_act(j, t, sub)
                    if 0 <= t - 2 < nk:
                        for j in J:
                            stage_a2(j, t - 2)
                    if 0 <= t - 1 < nk:
                        for j in J:
                            stage_dve1(j, t - 1)
                    if 0 <= t - 3 < nk:
                        for j in J:
                            stage_pv(j, t - 3)
                    if 0 <= t - 2 < nk:
                        for j in J:
                            stage_lt(j, t - 2)
                        for j in J:
                            stage_w(j, t - 2)
                    if 0 <= t - 1 < nk:
                        for j in J:
                            stage_u(j, t - 1)
            else:
                for t in range(nk + 1):
                    if t < nk:
                        for j in J:
                            stage_s(j, t)
                        for j in J:
                            stage_act(j, t)
                    if 0 <= t - 1 < nk:
                        for j in J:
                            stage_pv(j, t - 1)
            for j, (sl, h, g_, Kop, VH) in enumerate(jobs):
                ON = sl.ON.next()
                O = st[j]["O"]
                if m > 0:
                    rc = sl.rc
                    op("dve", lambda q, rc=rc, O=O: q.reciprocal(out=rc[64:128, :], in_=O[64:128, :]), reads=[O], writes=[rc])
                    op("dve", lambda q, rc=rc, O=O, ON=ON: q.tensor_tensor(out=ON[:], in0=O[0:64, :], in1=rc[64:128, :], op=ALU.mult), reads=[O, rc], writes=[ON])
                else:
                    op("dve", lambda q, O=O, ON=ON: q.tensor_copy(out=ON[:], in_=O[0:64, :]), reads=[O], writes=[ON])
                dma("act", OT[m * 512 + h * 64: m * 512 + (h + 1) * 64, g * 512:(g + 1) * 512], ON[:], reads=[ON])

        for m in range(3):
            R = RROWS[m]
            nv = 4 if NT >= 4 else 1
            step = NT // nv
            for i in range(nv):
                dma("sp", Vall[:, i * step:(i + 1) * step, :], VV[m][i * step * 128:(i + 1) * step * 128, :].rearrange("(c p) f -> p c f", p=128),
                    writes=[Vall] if i == 0 else (), accum=() if i == 0 else [Vall])
            for hp in range(4):
                pair = []
                for si in range(2):
                    h = hp * 2 + si
                    Kop = Koprot.next()
                    dma("sp", Kop[0:R, :], KOP[m][h], writes=[Kop])
                    VH = None
                    if m > 0:
                        VH = VHrot.next()
                        op("pool", lambda q, VH=VH, h=h: q.tensor_copy(out=VH[:, :, 0:64], in_=Vall[:, :, h * 64:(h + 1) * 64]), reads=[Vall], writes=[VH])
                    pair.append((h, Kop, VH))
                for g in range(NG):
                    run_jobs(m, [(slots[si], pair[si][0], g, pair[si][1], pair[si][2]) for si in range(2)])
        B.barrier()
        if stop_after == "B":
            break

        B.release(persist_mark)
        Wg = B.sb([128, 8, 3 * D], BF16, "Wg")
        Wbr = B.sb([128, 12, D], BF16, "Wbr")
        Wo = B.sb([128, 8, D], BF16, "Wo")
        stages = Rot([B.sb([128, 1024], F32, "stg") for _ in range(2)])
        load_weight(Wg, 8, 3 * D, lambda c, c0, c1: w_in[li, c * 128:(c + 1) * 128, OFF_G + c0:OFF_G + c1], Gt, stages)
        load_weight(Wbr, 12, D, lambda c, c0, c1: w_branch[li, c * 128:(c + 1) * 128, c0:c1], None, stages)
        load_weight(Wo, 8, D, lambda c, c0, c1: w_out[li, c * 128:(c + 1) * 128, c0:c1], None, stages)
        XGrot = Rot([B.sb([128, 4, D], F32, "XG") for _ in range(2)])
        hbrot = Rot([B.sb([128, D], BF16, "hb") for _ in range(1)])
        ssqrot = Rot([B.sb([128, 4], F32, "ssq") for _ in range(4)])
        hTrot = Rot([B.sb([128, 8, 512], BF16, "hT") for _ in range(2)])
        OTrot = Rot([B.sb([128, 12, 512], BF16, "OTg") for _ in range(2)])
        sgrot = Rot([B.sb([128, 512], F32, "sg") for _ in range(2)])
        tmprot = Rot([B.sb([128, 512], F32, "tmp") for _ in range(1)])
        accrot = Rot([B.sb([128, 512], F32, "acc") for _ in range(2)])
        mixrot = Rot([B.sb([128, 8, 512], BF16, "mixT") for _ in range(1)])
        psGrot = Rot(psf[0:2])
        psLrot = Rot(psf[2:4])
        psOrot = Rot(psf[4:6])
        prepped = {}

        def prepC(g):
            tok0 = g * 512
            XG = XGrot.next()
            dma("sp", XG[:], xsrc[tok0:tok0 + 512, :].rearrange("(t p) d -> p t d", p=128), writes=[XG])
            OTg = OTrot.next()
            dma("sp", OTg[:], OT[:, tok0:tok0 + 512].rearrange("(c p) t -> p c t", p=128), writes=[OTg])
            hT = hTrot.next()
            for t in range(4):
                norm_transpose((XG, XG[:, t, :]), lambda hT=hT, t=t: hT[:, :, t * 128:(t + 1) * 128], hT, ssqrot, hbrot, psb[0],
                               "dve" if t % 2 == 0 else "act")
            prepped[g] = (XG, OTg, hT)

        prepC(0)
        for g in range(NG):
            tok0 = g * 512
            XG, OTg, hT = prepped.pop(g)
            mixT = mixrot.next()
            for dc in range(8):
                if dc == 4 and g + 1 < NG:
                    prepC(g + 1)
                acc = accrot.next()
                for n in range(3):
                    psG = psGrot.next()
                    psL = psLrot.next()
                    for kc in range(8):
                        op("pe", lambda q, psG=psG, kc=kc, n=n, dc=dc, hT=hT: q.matmul(psG[:], lhsT=Wg[:, kc, n * D + dc * 128:n * D + (dc + 1) * 128], rhs=hT[:, kc, :], start=(kc == 0), stop=(kc == 7)),
                           reads=[Wg, hT], writes=[psG] if kc == 0 else (), accum=() if kc == 0 else [psG])
                    for wc in range(4):
                        op("pe", lambda q, psL=psL, wc=wc, n=n, dc=dc, OTg=OTg: q.matmul(psL[:], lhsT=Wbr[:, n * 4 + wc, dc * 128:(dc + 1) * 128], rhs=OTg[:, n * 4 + wc, :], start=(wc == 0), stop=(wc == 3)),
                           reads=[Wbr, OTg], writes=[psL] if wc == 0 else (), accum=() if wc == 0 else [psL])
                    sg = sgrot.next()
                    op("act", lambda q, sg=sg, psG=psG: q.activation(out=sg[:], in_=psG[:], func=AF.Sigmoid), reads=[psG], writes=[sg])
                    if n == 0:
                        op("dve", lambda q, acc=acc, sg=sg, psL=psL: q.tensor_tensor(out=acc[:], in0=psL[:], in1=sg[:], op=ALU.mult), reads=[psL, sg], writes=[acc])
                    else:
                        tmp = tmprot.next()
                        op("dve", lambda q, tmp=tmp, sg=sg, psL=psL: q.tensor_tensor(out=tmp[:], in0=psL[:], in1=sg[:], op=ALU.mult), reads=[psL, sg], writes=[tmp])
                        if n == 1:
                            op("dve", lambda q, acc=acc, tmp=tmp: q.tensor_tensor(out=acc[:], in0=acc[:], in1=tmp[:], op=ALU.add), reads=[tmp], writes=[acc])
                        else:
                            op("dve", lambda q, acc=acc, tmp=tmp, mixT=mixT, dc=dc: q.tensor_tensor(out=mixT[:, dc, :], in0=acc[:], in1=tmp[:], op=ALU.add), reads=[tmp, acc], accum=[mixT])
            for t in range(4):
                for half in range(2):
                    psO = psOrot.next()
                    for dc in range(8):
                        op("pe", lambda q, psO=psO, dc=dc, t=t, half=half, mixT=mixT: q.matmul(psO[:], lhsT=mixT[:, dc, t * 128:(t + 1) * 128], rhs=Wo[:, dc, half * 512:(half + 1) * 512], start=(dc == 0), stop=(dc == 7)),
                           reads=[mixT, Wo], writes=[psO] if dc == 0 else (), accum=() if dc == 0 else [psO])
                    op("dve", lambda q, psO=psO, XG=XG, t=t, half=half: q.tensor_tensor(out=XG[:, t, half * 512:(half + 1) * 512], in0=psO[:], in1=XG[:, t, half * 512:(half + 1) * 512], op=ALU.add),
                       reads=[psO], writes=[XG])
            dma("act", xres[tok0:tok0 + 512, :].rearrange("(t p) d -> p t d", p=128), XG[:], reads=[XG])
        B.barrier()
        if stop_after == "C":
            break

        B.release(persist_mark)
        Wup = B.sb([128, 8, DFF], BF16, "Wup")
        Wdn = B.sb([128, 32, D], BF16, "Wdn")
        stages = Rot([B.sb([128, 1024], F32, "stg") for _ in range(2)])
        load_weight(Wup, 8, DFF, lambda c, c0, c1: w_up[li, c * 128:(c + 1) * 128, c0:c1], G2t, stages)
        load_weight(Wdn, 32, D, lambda c, c0, c1: w_down[li, c * 128:(c + 1) * 128, c0:c1], None, stages)
        xrot = Rot([B.sb([128, D], F32, "xt") for _ in range(2)])
        hbrot = Rot([B.sb([128, D], BF16, "hb") for _ in range(2)])
        ssqrot = Rot([B.sb([128, 4], F32, "ssq") for _ in range(4)])
        h2Trot = Rot([B.sb([128, 8, 128], BF16, "h2T") for _ in range(2)])
        hidrot = Rot([B.sb([128, 32, 128], BF16, "hidT") for _ in range(2)])
        rrot = Rot([B.sb([128, 512], F32, "rl") for _ in range(2)])
        if last:
            NF = B.sb([128, D], F32, "NF")
            dma("sp", NF[:], norm_final, writes=[NF])
            yrot = Rot([B.sb([128, D], F32, "yt") for _ in range(2)])
            junk = B.sb([128, D], BF16, "junk")
        psUrot = Rot(psf[0:3])
        psDrot = Rot(psf[3:6])
        for tt in range(NT):
            xt = xrot.next()
            dma("sp", xt[:], xres[tt * 128:(tt + 1) * 128, :], writes=[xt])
            h2T = h2Trot.next()
            norm_transpose((xt, xt[:]), lambda h2T=h2T: h2T[:, :, :], h2T, ssqrot, hbrot, psb[0], "dve" if tt % 2 == 0 else "act")
            hid = hidrot.next()
            for fq in range(8):
                psU = psUrot.next()
                for j in range(4):
                    fc = fq * 4 + j
                    for kc in range(8):
                        first = (j == 0 and kc == 0)
                        op("pe", lambda q, psU=psU, j=j, fc=fc, kc=kc, h2T=h2T: q.matmul(psU[:, j * 128:(j + 1) * 128], lhsT=Wup[:, kc, fc * 128:(fc + 1) * 128], rhs=h2T[:, kc, :], start=(kc == 0), stop=(kc == 7)),
                           reads=[Wup, h2T], writes=[psU] if first else (), accum=() if first else [psU])
                rl = rrot.next()
                op("dve", lambda q, rl=rl, psU=psU: q.tensor_scalar(out=rl[:], in0=psU[:], scalar1=0.0, scalar2=None, op0=ALU.max), reads=[psU], writes=[rl])
                op("act", lambda q, rl=rl, hid=hid, fq=fq: q.activation(out=hid[:, fq * 4:(fq + 1) * 4, :], in_=rl[:, :].rearrange("p (j t) -> p j t", j=4), func=AF.Square), reads=[rl], accum=[hid])
            for half in range(2):
                psD = psDrot.next()
                for fc in range(32):
                    op("pe", lambda q, psD=psD, fc=fc, half=half, hid=hid: q.matmul(psD[:], lhsT=hid[:, fc, :], rhs=Wdn[:, fc, half * 512:(half + 1) * 512], start=(fc == 0), stop=(fc == 31)),
                       reads=[hid, Wdn], writes=[psD] if fc == 0 else (), accum=() if fc == 0 else [psD])
                op("dve", lambda q, psD=psD, xt=xt, half=half: q.tensor_tensor(out=xt[:, half * 512:(half + 1) * 512], in0=psD[:], in1=xt[:, half * 512:(half + 1) * 512], op=ALU.add),
                   reads=[psD], writes=[xt])
            if not last:
                xdst = xres if li < L - 1 else y
                dma("act", xdst[tt * 128:(tt + 1) * 128, :], xt[:], reads=[xt])
            else:
                if debug:
                    dma("act", xres[tt * 128:(tt + 1) * 128, :], xt[:], reads=[xt])
                ssq = ssqrot.next()
                yt = yrot.next()
                op("act", lambda q, xt=xt, ssq=ssq: q.activation(out=junk[:], in_=xt[:], func=AF.Square, accum_out=ssq[:, 0:1]), reads=[xt], writes=[junk, ssq])
                op("act", lambda q, ssq=ssq: q.activation(out=ssq[:, 1:2], in_=ssq[:, 0:1], func=AF.Sqrt, scale=1.0 / D, bias=epsb[:, 0:1]), reads=[ssq, epsb], accum=[ssq])
                op("dve", lambda q, ssq=ssq: q.reciprocal(out=ssq[:, 2:3], in_=ssq[:, 1:2]), reads=[ssq], accum=[ssq])
                op("dve", lambda q, xt=xt, ssq=ssq, yt=yt: q.scalar_tensor_tensor(out=yt[:], in0=xt[:], scalar=ssq[:, 2:3], in1=NF[:], op0=ALU.mult, op1=ALU.mult), reads=[xt, ssq, NF], writes=[yt], sw=[ssq])
                dma("act", y[tt * 128:(tt + 1) * 128, :], yt[:], reads=[yt])
        B.barrier()

    nsem = B._n_sems
    import contextlib
    with contextlib.ExitStack() as es:
        sems = [es.enter_context(nc.semaphore(f"s{i}")) for i in range(nsem)]
        B.sem_handles = sems
        block = es.enter_context(nc.Block())

        @block.sync
        def _(q):
            B.emit("sp", q)

        @block.tensor
        def _(q):
            B.emit("pe", q)

        @block.scalar
        def _(q):
            B.emit("act", q)

        @block.vector
        def _(q):
            B.emit("dve", q)

        @block.gpsimd
        def _(q):
            B.emit("pool", q)
    return nc


_BF = ml_dtypes.bfloat16
_NC_CACHE = {}


def make_consts(S):
    NB = 16
    c = {}
    c["c_ident"] = np.eye(128, dtype=np.float32).astype(_BF)
    j = np.arange(128)[:, None]
    k = np.arange(128)[None, :]
    c["c_ustrict"] = (-(j > k).astype(np.float32)).astype(_BF)
    c["c_nlt"] = (-(j <= k).astype(np.float32)).astype(_BF)
    p = np.arange(128)[:, None, None]
    v = np.arange(4)[None, :, None]
    f = np.arange(512)[None, None, :]
    c["c_maskaddlt"] = np.where((v * 128 + p) < f, 0.0, -BIG).astype(np.float32)
    c["c_maskadd"] = np.where((v * 128 + p) <= f, 0.0, -BIG).astype(np.float32)
    own = np.arange(NB)[:, None, None]
    n = np.arange(NB)[None, None, :]
    past = np.broadcast_to(n < own, (NB, 8, NB)).reshape(NB, 128)
    isown = np.broadcast_to(n == own, (NB, 8, NB)).reshape(NB, 128)
    c["c_pastadd"] = np.ascontiguousarray(np.broadcast_to(np.where(past, 0.0, -1e30).astype(np.float32)[None], (128, NB, 128)))
    c["c_validpast"] = np.ascontiguousarray(np.broadcast_to(past.astype(np.float32)[None], (128, NB, 128))).astype(_BF)
    c["c_isown"] = np.ascontiguousarray(np.broadcast_to(isown.astype(np.float32)[None], (128, NB, 128))).astype(_BF)
    c["c_foxq"] = np.ones((8, 3, S), np.float32).astype(_BF)
    slopes = (2.0 ** (-8.0 * np.arange(1, 9) / 8)).astype(np.float32)[:, None]
    pos = np.arange(S)[None, :]
    blk = (pos // 256).astype(np.float32)
    off = (pos % 256).astype(np.float32)
    mq = np.zeros((8, 4, S), np.float32)
    mq[:, 0] = -slopes * 256.0 * blk
    mq[:, 1] = -slopes * off
    mq[:, 2] = 1.0
    mq[:, 3] = 1.0
    c["c_mobq"] = mq.astype(_BF)
    mk = np.zeros((8, 20, S), np.float32)
    for nb in range(NB):
        mk[:, nb] = (pos // 256 == nb).astype(np.float32)
    mk[:, 16] = 1.0
    mk[:, 17] = 1.0
    mk[:, 18] = slopes * 256.0 * blk
    mk[:, 19] = slopes * off
    c["c_mobk"] = mk.astype(_BF)
    return c


def layer_inputs(l0, l1, norm_mix, w_in, b_forget, w_branch, w_out, norm_mlp, w_up, w_down, norm_final):
    f32 = np.float32
    L = l1 - l0
    d = {}
    d["norm_mix"] = np.ascontiguousarray(np.asarray(norm_mix, f32)[l0:l1].reshape(L, 8, 128).transpose(0, 2, 1))
    d["norm_mlp"] = np.ascontiguousarray(np.asarray(norm_mlp, f32)[l0:l1].reshape(L, 8, 128).transpose(0, 2, 1))
    d["w_in"] = np.ascontiguousarray(np.asarray(w_in, f32)[l0:l1])
    d["b_forget"] = np.ascontiguousarray(np.asarray(b_forget, f32)[l0:l1].reshape(L, 8, 1))
    d["w_branch"] = np.ascontiguousarray(np.asarray(w_branch, f32)[l0:l1].reshape(L, 3 * BW, D))
    d["w_out"] = np.ascontiguousarray(np.asarray(w_out, f32)[l0:l1])
    d["w_up"] = np.ascontiguousarray(np.asarray(w_up, f32)[l0:l1])
    d["w_down"] = np.ascontiguousarray(np.asarray(w_down, f32)[l0:l1])
    d["norm_final"] = np.ascontiguousarray(np.broadcast_to(np.asarray(norm_final, f32)[None, :], (128, D)))
    return d


def kernel(x, norm_mix, w_in, b_forget, w_branch, w_out, norm_mlp, w_up, w_down, norm_final):
    x = np.asarray(x, np.float32)
    Bsz, S, _ = x.shape
    L = int(np.asarray(norm_mix).shape[0])
    key = (S, L)
    if key not in _NC_CACHE:
        _NC_CACHE[key] = build_program(S, L, first_layer=0, n_layers_total=L)
    nc = _NC_CACHE[key]
    base = layer_inputs(0, L, norm_mix, w_in, b_forget, w_branch, w_out, norm_mlp, w_up, w_down, norm_final)
    base.update(make_consts(S))
    n_cores = 8
    in_maps = []
    for c in range(n_cores):
        d = dict(base)
        d["x"] = np.ascontiguousarray(x[c % Bsz])
        in_maps.append(d)
    res = run_bass_kernel_spmd(nc, in_maps, core_ids=list(range(n_cores)))
    out = np.stack([np.asarray(res.results[b]["y"], np.float32) for b in range(Bsz)], axis=0)
    return out
```

```python
import numpy as np
import ml_dtypes
import concourse.bass as bass
import concourse.mybir as mybir
from concourse.bass_utils import run_bass_kernel_spmd

F32 = mybir.dt.float32
BF16 = mybir.dt.bfloat16
AF = mybir.ActivationFunctionType
ALU = mybir.AluOpType
AX = mybir.AxisListType

D = 1024
HD = 64
NH = 8
BW = 512
DFF = 4096
IN_COLS = 9 * BW + NH + 3 * D
QKVF = 9 * BW + NH
OFF_Q = [0, 1536, 3080]
OFF_K = [512, 2048, 3592]
OFF_V = [1024, 2560, 4104]
OFF_F = 3072
OFF_G = 4616
RROWS = [64, 70, 84]
EPS = 1e-6
BIG = 30000.0
SBUF_BASE = 16640
SBUF_BYTES = 229000


class Buf:
    __slots__ = ("t", "w", "r", "name")

    def __init__(self, t, name=""):
        self.t = t
        self.w = {}
        self.r = {}
        self.name = name

    def __getitem__(self, idx):
        return self.t[idx]


class Eng:
    def __init__(self, name, sid):
        self.name = name
        self.sid = sid
        self.n = 0
        self.seen = {}
        self.prog = []


class Builder:
    def __init__(self, nc, n_dma_sp=12, n_dma_pool=2, n_dma_act=10):
        self.nc = nc
        self.sem_handles = []
        self.eng = {}
        self.dma_pools = {}
        self.dma_rr = {}
        self.sb_off = SBUF_BASE
        self.uid = 0
        self._n_sems = 5 + n_dma_sp + n_dma_pool + n_dma_act
        self._pool_sizes = {"sp": n_dma_sp, "pool": n_dma_pool, "act": n_dma_act}
        self.setup()

    def setup(self):
        i = 0
        for name in ["pe", "act", "dve", "pool", "sp"]:
            self.eng[name] = Eng(name, i)
            i += 1
        for q, n in self._pool_sizes.items():
            self.dma_pools[q] = [[i + j, 0] for j in range(n)]
            self.dma_rr[q] = 0
            i += n

    def sb(self, shape, dtype, name="t"):
        esz = 4 if dtype == F32 else 2
        nbytes = int(np.prod(shape[1:])) * esz
        off = (self.sb_off + 31) // 32 * 32
        assert off + nbytes <= SBUF_BYTES, f"SBUF overflow {name} {off}+{nbytes}"
        self.uid += 1
        t = self.nc.alloc_sbuf_tensor_at(f"{name}{self.uid}", list(shape), dtype, offset=off)
        self.sb_off = off + nbytes
        return Buf(t, name)

    def mark(self):
        return self.sb_off

    def release(self, mark):
        self.sb_off = mark

    def _need(self, reads, writes, accum):
        need = {}

        def merge(d):
            for k, v in d.items():
                if need.get(k, 0) < v:
                    need[k] = v

        for b in reads:
            merge(b.w)
        for b in writes:
            merge(b.w)
            merge(b.r)
        for b in accum:
            merge(b.r)
        return need

    def _emit_waits(self, e, need, own_ok=False):
        for sid, c in need.items():
            if sid == e.sid and not own_ok and e.name in ("pe", "sp"):
                continue
            if e.seen.get(sid, 0) < c:
                e.prog.append(("wait", sid, c))
                e.seen[sid] = c

    def op(self, en, fn, reads=(), writes=(), accum=(), sw=()):
        e = self.eng[en]
        self._emit_waits(e, self._need(reads, writes, accum))
        if sw:
            self._emit_waits(e, {k: v for b in sw for k, v in b.w.items() if k == e.sid}, own_ok=True)
        e.n += 1
        e.prog.append(("op", fn))
        for b in reads:
            b.r[e.sid] = e.n
        for b in writes:
            b.w = {e.sid: e.n}
            b.r = {}
        for b in accum:
            b.w[e.sid] = e.n

    def dma(self, qn, out_ap, in_ap, reads=(), writes=(), accum=()):
        e = self.eng[qn]
        pool = self.dma_pools[qn]
        s = pool[self.dma_rr[qn] % len(pool)]
        self.dma_rr[qn] += 1
        need = self._need(reads, writes, accum)
        if need.get(s[0], 0) < s[1]:
            need[s[0]] = s[1]
        self._emit_waits(e, need)
        s[1] += 16
        e.prog.append(("dma", out_ap, in_ap, s[0]))
        for b in reads:
            b.r[s[0]] = s[1]
        for b in writes:
            b.w = {s[0]: s[1]}
            b.r = {}
        for b in accum:
            b.w[s[0]] = s[1]

    def barrier(self):
        allc = {}
        for e in self.eng.values():
            allc[e.sid] = e.n
        for pool in self.dma_pools.values():
            for s in pool:
                allc[s[0]] = s[1]
        for e in self.eng.values():
            self._emit_waits(e, {k: v for k, v in allc.items() if v > 0})

    def emit(self, en, q):
        e = self.eng[en]
        sh = self.sem_handles
        mine = sh[e.sid]
        for it in e.prog:
            if it[0] == "wait":
                q.wait_ge(sh[it[1]], it[2])
            elif it[0] == "op":
                it[1](q).then_inc(mine, 1)
            else:
                q.dma_start(out=it[1], in_=it[2]).then_inc(sh[it[3]], 16)


class Rot:
    def __init__(self, bufs):
        self.bufs = bufs
        self.i = 0

    def next(self):
        b = self.bufs[self.i % len(self.bufs)]
        self.i += 1
        return b


def build_program(S, L, first_layer=0, n_layers_total=4, debug=False, stop_after=None):
    NT = S // 128
    NG = S // 512
    NB = 16
    nc = bass.Bass("TRN2", target_bir_lowering=False)
    dt = nc.dram_tensor
    x_in = dt("x", [S, D], F32, kind="ExternalInput").ap()
    norm_mix = dt("norm_mix", [L, 128, 8], F32, kind="ExternalInput").ap()
    w_in = dt("w_in", [L, D, IN_COLS], F32, kind="ExternalInput").ap()
    b_forget = dt("b_forget", [L, 8, 1], F32, kind="ExternalInput").ap()
    w_branch = dt("w_branch", [L, 3 * BW, D], F32, kind="ExternalInput").ap()
    w_out = dt("w_out", [L, D, D], F32, kind="ExternalInput").ap()
    norm_mlp = dt("norm_mlp", [L, 128, 8], F32, kind="ExternalInput").ap()
    w_up = dt("w_up", [L, D, DFF], F32, kind="ExternalInput").ap()
    w_down = dt("w_down", [L, DFF, D], F32, kind="ExternalInput").ap()
    norm_final = dt("norm_final", [128, D], F32, kind="ExternalInput").ap()
    c_ident = dt("c_ident", [128, 128], BF16, kind="ExternalInput").ap()
    c_ustrict = dt("c_ustrict", [128, 128], BF16, kind="ExternalInput").ap()
    c_nlt = dt("c_nlt", [128, 128], BF16, kind="ExternalInput").ap()
    c_maskaddlt = dt("c_maskaddlt", [128, 4, 512], F32, kind="ExternalInput").ap()
    c_maskadd = dt("c_maskadd", [128, 4, 512], F32, kind="ExternalInput").ap()
    c_pastadd = dt("c_pastadd", [128, NB, 128], F32, kind="ExternalInput").ap()
    c_validpast = dt("c_validpast", [128, NB, 128], BF16, kind="ExternalInput").ap()
    c_isown = dt("c_isown", [128, NB, 128], BF16, kind="ExternalInput").ap()
    c_foxq = dt("c_foxq", [8, 3, S], BF16, kind="ExternalInput").ap()
    c_mobq = dt("c_mobq", [8, 4, S], BF16, kind="ExternalInput").ap()
    c_mobk = dt("c_mobk", [8, 20, S], BF16, kind="ExternalInput").ap()
    y = dt("y", [S, D], F32, kind="ExternalOutput").ap()
    okind = "ExternalOutput" if debug else "Internal"
    xres = dt("xres", [S, D], F32, kind=okind).ap()
    QOP = [dt(f"qop{m}", [8, RROWS[m], S], BF16, kind=okind).ap() for m in range(3)]
    KOP = [dt(f"kop{m}", [8, RROWS[m], S], BF16, kind=okind).ap() for m in range(3)]
    VV = [dt(f"vv{m}", [S, BW], BF16, kind=okind).ap() for m in range(3)]
    OT = dt("ot", [3 * BW, S], BF16, kind=okind).ap()

    B = Builder(nc)
    op, dma = B.op, B.dma

    psf = [Buf(nc.alloc_psum_tensor(f"psf{i}", [128, 512], F32), f"psf{i}") for i in range(6)]
    psb = [Buf(nc.alloc_psum_tensor(f"psb{i}", [128, 1024], BF16), f"psb{i}") for i in range(2)]

    ident = B.sb([128, 128], BF16, "ident")
    nU = B.sb([128, 128], BF16, "nU")
    nLT = B.sb([128, 128], BF16, "nLT")
    maskadd = B.sb([128, 4, 512], F32, "maskadd")
    maskaddlt = B.sb([128, 4, 512], F32, "maskaddlt")
    KM = B.sb([128, 4, NB], F32, "KM")
    KMb = B.sb([128, 8, NB], BF16, "KMb")
    Gt = B.sb([128, 8], F32, "Gt")
    G2t = B.sb([128, 8], F32, "G2t")
    nbf = B.sb([8, 1], F32, "nbf")
    persist_mark = B.mark()

    dma("sp", ident[:], c_ident, writes=[ident])
    dma("sp", nU[:], c_ustrict, writes=[nU])
    dma("sp", nLT[:], c_nlt, writes=[nLT])
    dma("sp", maskadd[:], c_maskadd, writes=[maskadd])
    dma("sp", maskaddlt[:], c_maskaddlt, writes=[maskaddlt])
    op("pool", lambda q: q.memset(KM[:], 0.0), writes=[KM])
    op("pool", lambda q: q.memset(KMb[:], 0.0), writes=[KMb])
    dma("sp", QOP[1][:, 67:70, :], c_foxq)
    dma("sp", KOP[1][:, 64:67, :], c_foxq)
    dma("sp", QOP[2][:, 80:84, :], c_mobq)
    dma("sp", KOP[2][:, 64:84, :], c_mobk)
    B.barrier()

    cast_rr = [0]

    def cast_scaled(out_ap, in_ap, scal_ap, reads, accum):
        k = (0, 2)[cast_rr[0] % 2]
        cast_rr[0] += 1
        if scal_ap is None:
            if k == 0:
                op("dve", lambda q: q.tensor_copy(out=out_ap, in_=in_ap), reads=reads, accum=accum)
            elif k == 1:
                op("pool", lambda q: q.tensor_copy(out=out_ap, in_=in_ap), reads=reads, accum=accum)
            else:
                op("act", lambda q: q.copy(out=out_ap, in_=in_ap), reads=reads, accum=accum)
        else:
            if k == 0:
                op("dve", lambda q: q.tensor_scalar(out=out_ap, in0=in_ap, scalar1=scal_ap, scalar2=None, op0=ALU.mult), reads=reads, accum=accum)
            elif k == 1:
                op("pool", lambda q: q.tensor_scalar(out=out_ap, in0=in_ap, scalar1=scal_ap, scalar2=None, op0=ALU.mult), reads=reads, accum=accum)
            else:
                op("act", lambda q: q.mul(out=out_ap, in_=in_ap, mul=scal_ap), reads=reads, accum=accum)

    def load_weight(Wb, nchunk, ncols, src_fn, gt, stages, cw=1024):
        for c in range(nchunk):
            for c0 in range(0, ncols, cw):
                c1 = min(ncols, c0 + cw)
                st = stages.next()
                dma("sp", st[:, 0:c1 - c0], src_fn(c, c0, c1), writes=[st])
                sc = None if gt is None else gt[:, c:c + 1]
                rd = [st] if gt is None else [st, gt]
                cast_scaled(Wb[:, c, c0:c1], st[:, 0:c1 - c0], sc, rd, [Wb])

    def norm_transpose(xt, hT_ap_fn, hT_buf, ssq_rot, hb_rot, tp, evac_eng):
        ssq = ssq_rot.next()
        hb = hb_rot.next()
        op("act", lambda q: q.activation(out=hb[:], in_=xt_ap(xt), func=AF.Square, accum_out=ssq[:, 0:1]),
           reads=[xt[0]], writes=[hb, ssq])
        op("act", lambda q: q.activation(out=ssq[:, 1:2], in_=ssq[:, 0:1], func=AF.Sqrt, scale=1.0 / D, bias=epsb[:, 0:1]),
           reads=[ssq, epsb], accum=[ssq])
        op("dve", lambda q: q.reciprocal(out=ssq[:, 2:3], in_=ssq[:, 1:2]), reads=[ssq], accum=[ssq])
        op("act", lambda q: q.activation(out=hb[:], in_=xt_ap(xt), func=AF.Copy, scale=ssq[:, 2:3]),
           reads=[xt[0], ssq], writes=[hb])
        for kc in range(8):
            op("pe", lambda q, kc=kc: q.transpose(out=tp[:, kc * 128:(kc + 1) * 128], in_=hb[:, kc * 128:(kc + 1) * 128], identity=ident[:]),
               reads=[hb, ident], writes=[tp] if kc == 0 else (), accum=() if kc == 0 else [tp])
        src = tp[:, :].rearrange("p (k t) -> p k t", k=8)
        if evac_eng == "dve":
            op("dve", lambda q: q.tensor_copy(out=hT_ap_fn(), in_=src), reads=[tp], accum=[hT_buf])
        else:
            op("act", lambda q: q.copy(out=hT_ap_fn(), in_=src), reads=[tp], accum=[hT_buf])
        return ssq

    def xt_ap(xt):
        return xt[1]

    epsb = B.sb([128, 1], F32, "epsb")
    op("pool", lambda q: q.memset(epsb[:], EPS), writes=[epsb])
    persist_mark = B.mark()

    for li in range(L):
        if stop_after == "init":
            break
        lg = first_layer + li
        xsrc = x_in if li == 0 else xres
        last = (lg == n_layers_total - 1)
        B.release(persist_mark)
        dma("sp", Gt[:], norm_mix[li], writes=[Gt])
        dma("sp", G2t[:], norm_mlp[li], writes=[G2t])
        dma("sp", nbf[:], b_forget[li], writes=[nbf])
        op("dve", lambda q: q.tensor_scalar(out=nbf[:], in0=nbf[:], scalar1=-1.0, scalar2=None, op0=ALU.mult), reads=[nbf], writes=[nbf])
        Wb = B.sb([128, 8, QKVF], BF16, "Wb")
        stages = Rot([B.sb([128, 1024], F32, "stg") for _ in range(6)])
        load_weight(Wb, 8, QKVF, lambda c, c0, c1: w_in[li, c * 128:(c + 1) * 128, c0:c1], Gt, stages)
        SA = {"A0": 0, "A1": 1, "A2": 2, "A3": 3, "A4": 4, "A42": 4.2, "A44": 4.4, "A46": 4.6, "A48": 4.8}.get(stop_after, 9)
        xrot = Rot([B.sb([128, D], F32, "xt") for _ in range(3)])
        hbrot = Rot([B.sb([128, D], BF16, "hb") for _ in range(2)])
        ssqrot = Rot([B.sb([128, 4], F32, "ssq") for _ in range(4)])
        hTrot = Rot([B.sb([128, 8, 512], BF16, "hT") for _ in range(3)])
        evrot = Rot([B.sb([128, 512], BF16, "ev") for _ in range(4)])
        Qc = B.sb([128, 4, 512], BF16, "Qc")
        onesf = B.sb([8, 512], F32, "onesf")
        op("pool", lambda q: q.memset(onesf[:], 1.0), writes=[onesf])
        fe = B.sb([8, 512], F32, "fe")
        fsp = B.sb([8, 512], F32, "fsp")
        Crot = Rot([B.sb([8, 512], F32, "Cg") for _ in range(2)])
        r1 = B.sb([8, 512], F32, "r1")
        r2 = B.sb([8, 512], F32, "r2")
        CQt = B.sb([8, 3, 512], BF16, "CQt")
        CKt = B.sb([8, 3, 512], BF16, "CKt")
        pastadd = B.sb([128, NB, 128], F32, "pastadd")
        validpast = B.sb([128, NB, 128], BF16, "validpast")
        isown = B.sb([128, NB, 128], BF16, "isown")
        dma("sp", pastadd[:], c_pastadd, writes=[pastadd])
        dma("sp", validpast[:], c_validpast, writes=[validpast])
        dma("sp", isown[:], c_isown, writes=[isown])
        rm = B.sb([128, 128], F32, "rm")
        mx8 = B.sb([128, 8, 8], F32, "mx8")
        sel = B.sb([128, 128], F32, "sel")
        mbt = B.sb([128, 128], BF16, "mbt")
        MBT = B.sb([128, 512], BF16, "MBT")
        czero = B.sb([8, 1], F32, "czero")
        op("pool", lambda q: q.memset(czero[:], 0.0), writes=[czero])
        cprev = czero
        psrot = Rot(psf[0:4])
        ev_i = [0]

        def evac(out_ap, in_ap, scale, reads, writes):
            k = ev_i[0] % 2
            ev_i[0] += 1
            if k == 0:
                if scale is None:
                    op("dve", lambda q: q.tensor_copy(out=out_ap, in_=in_ap), reads=reads, writes=writes)
                else:
                    op("dve", lambda q: q.tensor_scalar(out=out_ap, in0=in_ap, scalar1=scale, scalar2=None, op0=ALU.mult), reads=reads, writes=writes)
            else:
                if scale is None:
                    op("act", lambda q: q.copy(out=out_ap, in_=in_ap), reads=reads, writes=writes)
                else:
                    op("act", lambda q: q.mul(out=out_ap, in_=in_ap, mul=scale), reads=reads, writes=writes)

        hTs = {}
        cstate = {'cprev': cprev}

        def prepA(g):
            tok0 = g * 512
            hT = hTrot.next()
            for t in range(4):
                xt = xrot.next()
                dma("sp", xt[:], xsrc[tok0 + t * 128: tok0 + (t + 1) * 128, :], writes=[xt])
                norm_transpose((xt, xt[:]), lambda hT=hT, t=t: hT[:, :, t * 128:(t + 1) * 128], hT, ssqrot, hbrot, psb[0],
                               "dve" if t % 2 == 0 else "act")
            hTs[g] = hT

        def proj_qk(g, ms):
            tok0 = g * 512
            hT = hTs[g]
            for m in ms:
                for which in range(2):
                    base = (OFF_Q if which == 0 else OFF_K)[m]
                    dst = (QOP if which == 0 else KOP)[m]
                    for cc in range(4):
                        ps = psrot.next()
                        col0 = base + cc * 128
                        for kc in range(8):
                            op("pe", lambda q, ps=ps, kc=kc, col0=col0, hT=hT: q.matmul(ps[:], lhsT=Wb[:, kc, col0:col0 + 128], rhs=hT[:, kc, :], start=(kc == 0), stop=(kc == 7)),
                               reads=[Wb, hT], writes=[ps] if kc == 0 else (), accum=() if kc == 0 else [ps])
                        if m == 2 and which == 0:
                            op("dve", lambda q, ps=ps, cc=cc: q.tensor_scalar(out=Qc[:, cc, :], in0=ps[:], scalar1=0.125, scalar2=None, op0=ALU.mult),
                               reads=[ps], accum=[Qc])
                            for hh in range(2):
                                dma("act", dst[cc * 2 + hh, 0:64, tok0:tok0 + 512], Qc[hh * 64:(hh + 1) * 64, cc, :], reads=[Qc])
                            continue
                        ev = evrot.next()
                        evac(ev[:], ps[:], 0.125 if which == 0 else None, [ps], [ev])
                        if m == 2 and which == 1:
                            op("dve", lambda q, ps=ps, cc=cc, g=g: q.tensor_reduce(out=KM[:, cc, 2 * g:2 * g + 2], in_=ps[:, :].rearrange("p (b t) -> p b t", b=2), axis=AX.X, op=ALU.add),
                               reads=[ps], accum=[KM])
                            for hh in range(2):
                                op("dve", lambda q, cc=cc, g=g, hh=hh: q.tensor_copy(out=KMb[hh * 64:(hh + 1) * 64, cc * 2 + hh, 2 * g:2 * g + 2], in_=KM[hh * 64:(hh + 1) * 64, cc, 2 * g:2 * g + 2]),
                                   reads=[KM], accum=[KMb])
                        for hh in range(2):
                            dma("act", dst[cc * 2 + hh, 0:64, tok0:tok0 + 512], ev[hh * 64:(hh + 1) * 64, :], reads=[ev])

        def proj_v(g):
            tok0 = g * 512
            hT = hTs[g]
            for m in range(3):
                for t in range(4):
                    ps = psrot.next()
                    for kc in range(8):
                        op("pe", lambda q, ps=ps, kc=kc, m=m, t=t, hT=hT: q.matmul(ps[:], lhsT=hT[:, kc, t * 128:(t + 1) * 128], rhs=Wb[:, kc, OFF_V[m]:OFF_V[m] + 512], start=(kc == 0), stop=(kc == 7)),
                           reads=[Wb, hT], writes=[ps] if kc == 0 else (), accum=() if kc == 0 else [ps])
                    ev = evrot.next()
                    evac(ev[:], ps[:], None, [ps], [ev])
                    dma("act", VV[m][tok0 + t * 128: tok0 + (t + 1) * 128, :], ev[:], reads=[ev])

        def tailA(g):
            tok0 = g * 512
            hT = hTs[g]
            cprev = cstate['cprev']
            ps = psrot.next()
            for kc in range(8):
                op("pe", lambda q, ps=ps, kc=kc, hT=hT: q.matmul(ps[0:8, :], lhsT=Wb[:, kc, OFF_F:OFF_F + 8], rhs=hT[:, kc, :], start=(kc == 0), stop=(kc == 7)),
                   reads=[Wb, hT], writes=[ps] if kc == 0 else (), accum=() if kc == 0 else [ps])
            op("act", lambda q, ps=ps: q.activation(out=fe[:], in_=ps[0:8, :], func=AF.Exp, scale=-1.0, bias=nbf[:, 0:1]), reads=[ps, nbf], writes=[fe])
            op("act", lambda q: q.activation(out=fsp[:], in_=fe[:], func=AF.Ln, bias=1.0), reads=[fe], writes=[fsp])
            Cg = Crot.next()
            init_ap = cprev[:, 0:1] if cprev is czero else cprev[:, 511:512]
            op("dve", lambda q, Cg=Cg, init_ap=init_ap, onesf=onesf, fsp=fsp: q.tensor_tensor_scan(out=Cg[:], data0=onesf[:], data1=fsp[:], initial=init_ap, op0=ALU.mult, op1=ALU.subtract),
               reads=[onesf, fsp, cprev], writes=[Cg], sw=[cprev])
            cstate['cprev'] = Cg
            cprev = Cg
            op("dve", lambda q, Cg=Cg: q.tensor_copy(out=CQt[:, 0, :], in_=Cg[:]), reads=[Cg], writes=[CQt])
            op("dve", lambda q, Cg=Cg: q.tensor_tensor(out=r1[:], in0=Cg[:], in1=CQt[:, 0, :], op=ALU.subtract), reads=[Cg, CQt], writes=[r1])
            op("dve", lambda q: q.tensor_copy(out=CQt[:, 1, :], in_=r1[:]), reads=[r1], accum=[CQt])
            op("dve", lambda q: q.tensor_tensor(out=r2[:], in0=r1[:], in1=CQt[:, 1, :], op=ALU.subtract), reads=[r1, CQt], writes=[r2])
            op("dve", lambda q: q.tensor_copy(out=CQt[:, 2, :], in_=r2[:]), reads=[r2], accum=[CQt])
            op("dve", lambda q: q.tensor_scalar(out=CKt[:], in0=CQt[:], scalar1=-1.0, scalar2=None, op0=ALU.mult), reads=[CQt], writes=[CKt])
            dma("act", QOP[1][:, 64:67, tok0:tok0 + 512], CQt[:], reads=[CQt])
            dma("act", KOP[1][:, 67:70, tok0:tok0 + 512], CKt[:], reads=[CKt])
            for t in range(4):
                own = (tok0 + t * 128) // 256
                ps = psrot.next()
                for h in range(8):
                    pb = (h % 2) * 64
                    op("pe", lambda q, ps=ps, h=h, pb=pb, t=t: q.matmul(ps[:, h * 16:(h + 1) * 16], lhsT=Qc[:, h // 2, t * 128:(t + 1) * 128], rhs=KMb[:, h, :], start=True, stop=True),
                       reads=[Qc, KMb], writes=[ps] if h == 0 else (), accum=() if h == 0 else [ps])
                op("dve", lambda q, ps=ps, own=own: q.tensor_tensor(out=rm[:], in0=ps[:, 0:128], in1=pastadd[:, own, :], op=ALU.add), reads=[ps, pastadd], writes=[rm])
                for h in range(8):
                    op("dve", lambda q, h=h: q.max(out=mx8[:, h, :], in_=rm[:, h * 16:(h + 1) * 16]), reads=[rm], writes=[mx8] if h == 0 else (), accum=() if h == 0 else [mx8])
                for h in range(8):
                    op("dve", lambda q, h=h: q.tensor_scalar(out=sel[:, h * 16:(h + 1) * 16], in0=rm[:, h * 16:(h + 1) * 16], scalar1=mx8[:, h, 2:3], scalar2=None, op0=ALU.is_ge),
                       reads=[rm, mx8], writes=[sel] if h == 0 else (), accum=() if h == 0 else [sel], sw=[mx8])
                op("dve", lambda q, own=own: q.tensor_tensor(out=sel[:], in0=sel[:], in1=validpast[:, own, :], op=ALU.mult), reads=[validpast], writes=[sel])
                op("dve", lambda q, own=own: q.tensor_tensor(out=sel[:], in0=sel[:], in1=isown[:, own, :], op=ALU.add), reads=[isown], writes=[sel])
                op("dve", lambda q: q.tensor_scalar(out=mbt[:], in0=sel[:], scalar1=BIG, scalar2=-BIG, op0=ALU.mult, op1=ALU.add), reads=[sel], writes=[mbt])
                tp = psb[1]
                op("pe", lambda q, tp=tp: q.transpose(out=tp[:, 0:128], in_=mbt[:], identity=ident[:]), reads=[mbt, ident], writes=[tp])
                op("act", lambda q, tp=tp, t=t: q.copy(out=MBT[:, t * 128:(t + 1) * 128], in_=tp[:, 0:128]), reads=[tp], accum=[MBT])
            for h in range(8):
                dma("act", QOP[2][h, 64:80, tok0:tok0 + 512], MBT[h * 16:(h + 1) * 16, :], reads=[MBT])

        prepA(0)
        for g in range(NG):
            proj_qk(g, [0])
            if g + 1 < NG:
                prepA(g + 1)
            proj_qk(g, [1])
            if g > 0:
                tailA(g - 1)
            proj_qk(g, [2])
            proj_v(g)
        tailA(NG - 1)
        B.barrier()
        if stop_after is not None and stop_after.startswith("A"):
            break

        B.release(persist_mark)
        Vall = B.sb([128, NT, 512], BF16, "Vall")
        VHrot = Rot([B.sb([128, NT, 128], BF16, "VH") for _ in range(4)])
        for vb in VHrot.bufs:
            op("pool", lambda q, vb=vb: q.memset(vb[:], 1.0), writes=[vb])
        Koprot = Rot([B.sb([84, S], BF16, "Kop") for _ in range(4)])

        class Slot:
            pass

        slots = []
        for si in range(2):
            sl = Slot()
            sl.Qop = Rot([B.sb([84, 512], BF16, "Qop") for _ in range(2)])
            sl.Zm = Rot([B.sb([128, 512], F32, "Zm") for _ in range(2)])
            sl.E = Rot([B.sb([128, 512], F32, "E") for _ in range(2)])
            sl.SPt = Rot([B.sb([128, 512], F32, "SPt") for _ in range(2)])
            sl.SPh = Rot([B.sb([128, 512], BF16, "SPh") for _ in range(4)])
            sl.SPl = Rot([B.sb([128, 512], BF16, "SPl") for _ in range(3)])
            sl.A = Rot([B.sb([128, 512], F32, "A") for _ in range(3)])
            sl.W = Rot([B.sb([128, 512], BF16, "W") for _ in range(3)])
            sl.rc = B.sb([128, 512], F32, "rc")
            sl.ON = Rot([B.sb([64, 512], BF16, "ON") for _ in range(2)])
            sl.Zr = Rot([psf[si], Buf(psb[si].t[:, :].bitcast(F32), f"psbf{si}")])
            sl.ACC = psf[2 + si]
            sl.O = psf[4 + si]
            sl.Orot = Rot([psf[4 + si], psf[2 + si]])
            slots.append(sl)

        def run_jobs(m, jobs):
            R = RROWS[m]
            g = jobs[0][2]
            nk = 4 * (g + 1)
            st = []
            for (sl, h, g_, Kop, VH) in jobs:
                Qop = sl.Qop.next()
                dma("sp", Qop[0:R, :], QOP[m][h, :, g * 512:(g + 1) * 512], writes=[Qop])
                st.append({"Qop": Qop, "tiles": {}, "O": sl.O if m == 0 else sl.Orot.next()})

            def kc_of(i):
                return nk - 1 - i if m == 0 else i

            def stage_s(j, i):
                sl, h, g_, Kop, VH = jobs[j]
                Qop = st[j]["Qop"]
                kc = kc_of(i)
                v = kc - 4 * g
                T = {}
                st[j]["tiles"][i] = T
                Z = sl.Zr.next()
                op("pe", lambda q, Z=Z, Kop=Kop, Qop=Qop, kc=kc: q.matmul(Z[:], lhsT=Kop[0:R, kc * 128:(kc + 1) * 128], rhs=Qop[0:R, :], start=True, stop=True),
                   reads=[Kop, Qop], writes=[Z])
                T["zsrc"], T["zb"] = Z[:], Z
                if v >= 0:
                    Zm = sl.Zm.next()
                    madd = maskaddlt if m == 0 else maskadd
                    op("dve", lambda q, Zm=Zm, Z=Z, v=v, madd=madd: q.tensor_tensor(out=Zm[:], in0=Z[:], in1=madd[:, v, :], op=ALU.add), reads=[Z, madd], writes=[Zm])
                    T["zsrc"], T["zb"] = Zm[:], Zm

            def stage_act(j, i, sub=0):
                sl, h, g_, Kop, VH = jobs[j]
                T = st[j]["tiles"][i]
                zsrc, zb = T["zsrc"], T["zb"]
                if m > 0:
                    W = sl.W.next()
                    op("act", lambda q, W=W, zsrc=zsrc: q.activation(out=W[:], in_=zsrc, func=AF.Exp), reads=[zb], writes=[W])
                    T["W"] = W
                    return
                if sub == 0:
                    E = sl.E.next()
                    op("act", lambda q, E=E, zsrc=zsrc: q.activation(out=E[:], in_=zsrc, func=AF.Exp), reads=[zb], writes=[E])
                    T["E"] = E
                elif sub == 1:
                    E = T["E"]
                    SPt = sl.SPt.next()
                    op("act", lambda q, E=E, SPt=SPt: q.activation(out=SPt[:], in_=E[:], func=AF.Ln, bias=1.0), reads=[E], writes=[SPt])
                    T["SPt"] = SPt
                else:
                    SPt = T["SPt"]
                    SPh = sl.SPh.next()
                    op("act", lambda q, SPt=SPt, SPh=SPh: q.copy(out=SPh[:], in_=SPt[:]), reads=[SPt], writes=[SPh])
                    T["SPh"] = SPh

            def stage_dve1(j, i):
                sl, h, g_, Kop, VH = jobs[j]
                T = st[j]["tiles"][i]
                zsrc, zb, SPt, SPh = T["zsrc"], T["zb"], T["SPt"], T["SPh"]
                SPl = sl.SPl.next()
                A = sl.A.next()
                op("dve", lambda q, SPt=SPt, SPh=SPh, SPl=SPl: q.tensor_tensor(out=SPl[:], in0=SPt[:], in1=SPh[:], op=ALU.subtract), reads=[SPt, SPh], writes=[SPl])
                op("dve", lambda q, A=A, zsrc=zsrc, SPt=SPt: q.tensor_tensor(out=A[:], in0=zsrc, in1=SPt[:], op=ALU.subtract), reads=[zb, SPt], writes=[A])
                T.update(SPl=SPl, A=A)

            def stage_u(j, i):
                sl, h, g_, Kop, VH = jobs[j]
                T = st[j]["tiles"][i]
                ACC = sl.ACC
                SPh, SPl = T["SPh"], T["SPl"]
                lastc = (i == nk - 1)
                op("pe", lambda q, ACC=ACC, SPh=SPh, i=i: q.matmul(ACC[:], lhsT=nU[:], rhs=SPh[:], start=(i == 0), stop=False), reads=[nU, SPh],
                   writes=[ACC] if i == 0 else (), accum=() if i == 0 else [ACC])
                op("pe", lambda q, ACC=ACC, SPl=SPl, lastc=lastc: q.matmul(ACC[:], lhsT=nU[:], rhs=SPl[:], start=False, stop=lastc), reads=[nU, SPl], accum=[ACC])

            def stage_a2(j, i):
                sl, h, g_, Kop, VH = jobs[j]
                T = st[j]["tiles"][i]
                ACC = sl.ACC
                A = T["A"]
                op("dve", lambda q, A=A, ACC=ACC: q.tensor_tensor(out=A[:], in0=ACC[:], in1=A[:], op=ALU.add), reads=[ACC], writes=[A])

            def stage_w(j, i):
                sl, h, g_, Kop, VH = jobs[j]
                T = st[j]["tiles"][i]
                A = T["A"]
                W = sl.W.next()
                op("act", lambda q, A=A, W=W: q.activation(out=W[:], in_=A[:], func=AF.Exp), reads=[A], writes=[W])
                T["W"] = W

            def stage_lt(j, i):
                sl, h, g_, Kop, VH = jobs[j]
                if i == nk - 1:
                    return
                T = st[j]["tiles"][i]
                ACC = sl.ACC
                SPh, SPl = T["SPh"], T["SPl"]
                op("pe", lambda q, ACC=ACC, SPh=SPh: q.matmul(ACC[:], lhsT=nLT[:], rhs=SPh[:], start=False, stop=False), reads=[nLT, SPh], accum=[ACC])
                op("pe", lambda q, ACC=ACC, SPl=SPl: q.matmul(ACC[:], lhsT=nLT[:], rhs=SPl[:], start=False, stop=False), reads=[nLT, SPl], accum=[ACC])

            def stage_pv(j, i):
                sl, h, g_, Kop, VH = jobs[j]
                T = st[j]["tiles"].pop(i)
                W = T["W"]
                kc = kc_of(i)
                O = st[j]["O"]
                if m == 0:
                    op("pe", lambda q, O=O, W=W, kc=kc, h=h, i=i: q.matmul(O[0:64, :], lhsT=Vall[:, kc, h * 64:(h + 1) * 64], rhs=W[:], start=(i == 0), stop=(i == nk - 1)),
                       reads=[Vall, W], writes=[O] if i == 0 else (), accum=() if i == 0 else [O])
                else:
                    op("pe", lambda q, O=O, W=W, kc=kc, VH=VH, i=i: q.matmul(O[:], lhsT=VH[:, kc, :], rhs=W[:], start=(i == 0), stop=(i == nk - 1)),
                       reads=[VH, W], writes=[O] if i == 0 else (), accum=() if i == 0 else [O])

            nj = len(jobs)
            J = range(nj)
            if m == 0:
                for t in range(nk + 3):
                    if t < nk:
                        for j in J:
                            stage_s(j, t)
                        for sub in range(3):
                            for j in J:
                                stage_act(j, t, sub)
                    if 0 <= t - 2 < nk:
                        for j in J:
                            stage_a2(j, t - 2)
                    if 0 <= t - 1 < nk:
                        for j in J:
                            stage_dve1(j, t - 1)
                    if 0 <= t - 3 < nk:
                        for j in J:
                            stage_pv(j, t - 3)
                    if 0 <= t - 2 < nk:
                        for j in J:
                            stage_lt(j, t - 2)
                        for j in J:
                            stage_w(j, t - 2)
                    if 0 <= t - 1 < nk:
                        for j in J:
                            stage_u(j, t - 1)
            else:
                for t in range(nk + 1):
                    if t < nk:
                        for j in J:
                            stage_s(j, t)
                        for j in J:
                            stage_act(j, t)
                    if 0 <= t - 1 < nk:
                        for j in J:
                            stage_pv(j, t - 1)
            for j, (sl, h, g_, Kop, VH) in enumerate(jobs):
                ON = sl.ON.next()
                O = st[j]["O"]
                if m > 0:
                    rc = sl.rc
                    op("dve", lambda q, rc=rc, O=O: q.reciprocal(out=rc[64:128, :], in_=O[64:128, :]), reads=[O], writes=[rc])
                    op("dve", lambda q, rc=rc, O=O, ON=ON: q.tensor_tensor(out=ON[:], in0=O[0:64, :], in1=rc[64:128, :], op=ALU.mult), reads=[O, rc], writes=[ON])
                else:
                    op("dve", lambda q, O=O, ON=ON: q.tensor_copy(out=ON[:], in_=O[0:64, :]), reads=[O], writes=[ON])
                dma("act", OT[m * 512 + h * 64: m * 512 + (h + 1) * 64, g * 512:(g + 1) * 512], ON[:], reads=[ON])

        for m in range(3):
            R = RROWS[m]
            nv = 4 if NT >= 4 else 1
            step = NT // nv
            for i in range(nv):
                dma("sp", Vall[:, i * step:(i + 1) * step, :], VV[m][i * step * 128:(i + 1) * step * 128, :].rearrange("(c p) f -> p c f", p=128),
                    writes=[Vall] if i == 0 else (), accum=() if i == 0 else [Vall])
            for hp in range(4):
                pair = []
                for si in range(2):
                    h = hp * 2 + si
                    Kop = Koprot.next()
                    dma("sp", Kop[0:R, :], KOP[m][h], writes=[Kop])
                    VH = None
                    if m > 0:
                        VH = VHrot.next()
                        op("pool", lambda q, VH=VH, h=h: q.tensor_copy(out=VH[:, :, 0:64], in_=Vall[:, :, h * 64:(h + 1) * 64]), reads=[Vall], writes=[VH])
                    pair.append((h, Kop, VH))
                for g in range(NG):
                    run_jobs(m, [(slots[si], pair[si][0], g, pair[si][1], pair[si][2]) for si in range(2)])
        B.barrier()
        if stop_after == "B":
            break

        B.release(persist_mark)
        Wg = B.sb([128, 8, 3 * D], BF16, "Wg")
        Wbr = B.sb([128, 12, D], BF16, "Wbr")
        Wo = B.sb([128, 8, D], BF16, "Wo")
        stages = Rot([B.sb([128, 1024], F32, "stg") for _ in range(2)])
        load_weight(Wg, 8, 3 * D, lambda c, c0, c1: w_in[li, c * 128:(c + 1) * 128, OFF_G + c0:OFF_G + c1], Gt, stages)
        load_weight(Wbr, 12, D, lambda c, c0, c1: w_branch[li, c * 128:(c + 1) * 128, c0:c1], None, stages)
        load_weight(Wo, 8, D, lambda c, c0, c1: w_out[li, c * 128:(c + 1) * 128, c0:c1], None, stages)
        XGrot = Rot([B.sb([128, 4, D], F32, "XG") for _ in range(2)])
        hbrot = Rot([B.sb([128, D], BF16, "hb") for _ in range(1)])
        ssqrot = Rot([B.sb([128, 4], F32, "ssq") for _ in range(4)])
        hTrot = Rot([B.sb([128, 8, 512], BF16, "hT") for _ in range(2)])
        OTrot = Rot([B.sb([128, 12, 512], BF16, "OTg") for _ in range(2)])
        sgrot = Rot([B.sb([128, 512], F32, "sg") for _ in range(2)])
        tmprot = Rot([B.sb([128, 512], F32, "tmp") for _ in range(1)])
        accrot = Rot([B.sb([128, 512], F32, "acc") for _ in range(2)])
        mixrot = Rot([B.sb([128, 8, 512], BF16, "mixT") for _ in range(1)])
        psGrot = Rot(psf[0:2])
        psLrot = Rot(psf[2:4])
        psOrot = Rot(psf[4:6])
        prepped = {}

        def prepC(g):
            tok0 = g * 512
            XG = XGrot.next()
            dma("sp", XG[:], xsrc[tok0:tok0 + 512, :].rearrange("(t p) d -> p t d", p=128), writes=[XG])
            OTg = OTrot.next()
            dma("sp", OTg[:], OT[:, tok0:tok0 + 512].rearrange("(c p) t -> p c t", p=128), writes=[OTg])
            hT = hTrot.next()
            for t in range(4):
                norm_transpose((XG, XG[:, t, :]), lambda hT=hT, t=t: hT[:, :, t * 128:(t + 1) * 128], hT, ssqrot, hbrot, psb[0],
                               "dve" if t % 2 == 0 else "act")
            prepped[g] = (XG, OTg, hT)

        prepC(0)
        for g in range(NG):
            tok0 = g * 512
            XG, OTg, hT = prepped.pop(g)
            mixT = mixrot.next()
            for dc in range(8):
                if dc == 4 and g + 1 < NG:
                    prepC(g + 1)
                acc = accrot.next()
                for n in range(3):
                    psG = psGrot.next()
                    psL = psLrot.next()
                    for kc in range(8):
                        op("pe", lambda q, psG=psG, kc=kc, n=n, dc=dc, hT=hT: q.matmul(psG[:], lhsT=Wg[:, kc, n * D + dc * 128:n * D + (dc + 1) * 128], rhs=hT[:, kc, :], start=(kc == 0), stop=(kc == 7)),
                           reads=[Wg, hT], writes=[psG] if kc == 0 else (), accum=() if kc == 0 else [psG])
                    for wc in range(4):
                        op("pe", lambda q, psL=psL, wc=wc, n=n, dc=dc, OTg=OTg: q.matmul(psL[:], lhsT=Wbr[:, n * 4 + wc, dc * 128:(dc + 1) * 128], rhs=OTg[:, n * 4 + wc, :], start=(wc == 0), stop=(wc == 3)),
                           reads=[Wbr, OTg], writes=[psL] if wc == 0 else (), accum=() if wc == 0 else [psL])
                    sg = sgrot.next()
                    op("act", lambda q, sg=sg, psG=psG: q.activation(out=sg[:], in_=psG[:], func=AF.Sigmoid), reads=[psG], writes=[sg])
                    if n == 0:
                        op("dve", lambda q, acc=acc, sg=sg, psL=psL: q.tensor_tensor(out=acc[:], in0=psL[:], in1=sg[:], op=ALU.mult), reads=[psL, sg], writes=[acc])
                    else:
                        tmp = tmprot.next()
                        op("dve", lambda q, tmp=tmp, sg=sg, psL=psL: q.tensor_tensor(out=tmp[:], in0=psL[:], in1=sg[:], op=ALU.mult), reads=[psL, sg], writes=[tmp])
                        if n == 1:
                            op("dve", lambda q, acc=acc, tmp=tmp: q.tensor_tensor(out=acc[:], in0=acc[:], in1=tmp[:], op=ALU.add), reads=[tmp], writes=[acc])
                        else:
                            op("dve", lambda q, acc=acc, tmp=tmp, mixT=mixT, dc=dc: q.tensor_tensor(out=mixT[:, dc, :], in0=acc[:], in1=tmp[:], op=ALU.add), reads=[tmp, acc], accum=[mixT])
            for t in range(4):
                for half in range(2):
                    psO = psOrot.next()
                    for dc in range(8):
                        op("pe", lambda q, psO=psO, dc=dc, t=t, half=half, mixT=mixT: q.matmul(psO[:], lhsT=mixT[:, dc, t * 128:(t + 1) * 128], rhs=Wo[:, dc, half * 512:(half + 1) * 512], start=(dc == 0), stop=(dc == 7)),
                           reads=[mixT, Wo], writes=[psO] if dc == 0 else (), accum=() if dc == 0 else [psO])
                    op("dve", lambda q, psO=psO, XG=XG, t=t, half=half: q.tensor_tensor(out=XG[:, t, half * 512:(half + 1) * 512], in0=psO[:], in1=XG[:, t, half * 512:(half + 1) * 512], op=ALU.add),
                       reads=[psO], writes=[XG])
            dma("act", xres[tok0:tok0 + 512, :].rearrange("(t p) d -> p t d", p=128), XG[:], reads=[XG])
        B.barrier()
        if stop_after == "C":
            break

        B.release(persist_mark)
        Wup = B.sb([128, 8, DFF], BF16, "Wup")
        Wdn = B.sb([128, 32, D], BF16, "Wdn")
        stages = Rot([B.sb([128, 1024], F32, "stg") for _ in range(2)])
        load_weight(Wup, 8, DFF, lambda c, c0, c1: w_up[li, c * 128:(c + 1) * 128, c0:c1], G2t, stages)
        load_weight(Wdn, 32, D, lambda c, c0, c1: w_down[li, c * 128:(c + 1) * 128, c0:c1], None, stages)
        xrot = Rot([B.sb([128, D], F32, "xt") for _ in range(2)])
        hbrot = Rot([B.sb([128, D], BF16, "hb") for _ in range(2)])
        ssqrot = Rot([B.sb([128, 4], F32, "ssq") for _ in range(4)])
        h2Trot = Rot([B.sb([128, 8, 128], BF16, "h2T") for _ in range(2)])
        hidrot = Rot([B.sb([128, 32, 128], BF16, "hidT") for _ in range(2)])
        rrot = Rot([B.sb([128, 512], F32, "rl") for _ in range(2)])
        if last:
            NF = B.sb([128, D], F32, "NF")
            dma("sp", NF[:], norm_final, writes=[NF])
            yrot = Rot([B.sb([128, D], F32, "yt") for _ in range(2)])
            junk = B.sb([128, D], BF16, "junk")
        psUrot = Rot(psf[0:3])
        psDrot = Rot(psf[3:6])
        for tt in range(NT):
            xt = xrot.next()
            dma("sp", xt[:], xres[tt * 128:(tt + 1) * 128, :], writes=[xt])
            h2T = h2Trot.next()
            norm_transpose((xt, xt[:]), lambda h2T=h2T: h2T[:, :, :], h2T, ssqrot, hbrot, psb[0], "dve" if tt % 2 == 0 else "act")
            hid = hidrot.next()
            for fq in range(8):
                psU = psUrot.next()
                for j in range(4):
                    fc = fq * 4 + j
                    for kc in range(8):
                        first = (j == 0 and kc == 0)
                        op("pe", lambda q, psU=psU, j=j, fc=fc, kc=kc, h2T=h2T: q.matmul(psU[:, j * 128:(j + 1) * 128], lhsT=Wup[:, kc, fc * 128:(fc + 1) * 128], rhs=h2T[:, kc, :], start=(kc == 0), stop=(kc == 7)),
                           reads=[Wup, h2T], writes=[psU] if first else (), accum=() if first else [psU])
                rl = rrot.next()
                op("dve", lambda q, rl=rl, psU=psU: q.tensor_scalar(out=rl[:], in0=psU[:], scalar1=0.0, scalar2=None, op0=ALU.max), reads=[psU], writes=[rl])
                op("act", lambda q, rl=rl, hid=hid, fq=fq: q.activation(out=hid[:, fq * 4:(fq + 1) * 4, :], in_=rl[:, :].rearrange("p (j t) -> p j t", j=4), func=AF.Square), reads=[rl], accum=[hid])
            for half in range(2):
                psD = psDrot.next()
                for fc in range(32):
                    op("pe", lambda q, psD=psD, fc=fc, half=half, hid=hid: q.matmul(psD[:], lhsT=hid[:, fc, :], rhs=Wdn[:, fc, half * 512:(half + 1) * 512], start=(fc == 0), stop=(fc == 31)),
                       reads=[hid, Wdn], writes=[psD] if fc == 0 else (), accum=() if fc == 0 else [psD])
                op("dve", lambda q, psD=psD, xt=xt, half=half: q.tensor_tensor(out=xt[:, half * 512:(half + 1) * 512], in0=psD[:], in1=xt[:, half * 512:(half + 1) * 512], op=ALU.add),
                   reads=[psD], writes=[xt])
            if not last:
                xdst = xres if li < L - 1 else y
                dma("act", xdst[tt * 128:(tt + 1) * 128, :], xt[:], reads=[xt])
            else:
                if debug:
                    dma("act", xres[tt * 128:(tt + 1) * 128, :], xt[:], reads=[xt])
                ssq = ssqrot.next()
                yt = yrot.next()
                op("act", lambda q, xt=xt, ssq=ssq: q.activation(out=junk[:], in_=xt[:], func=AF.Square, accum_out=ssq[:, 0:1]), reads=[xt], writes=[junk, ssq])
                op("act", lambda q, ssq=ssq: q.activation(out=ssq[:, 1:2], in_=ssq[:, 0:1], func=AF.Sqrt, scale=1.0 / D, bias=epsb[:, 0:1]), reads=[ssq, epsb], accum=[ssq])
                op("dve", lambda q, ssq=ssq: q.reciprocal(out=ssq[:, 2:3], in_=ssq[:, 1:2]), reads=[ssq], accum=[ssq])
                op("dve", lambda q, xt=xt, ssq=ssq, yt=yt: q.scalar_tensor_tensor(out=yt[:], in0=xt[:], scalar=ssq[:, 2:3], in1=NF[:], op0=ALU.mult, op1=ALU.mult), reads=[xt, ssq, NF], writes=[yt], sw=[ssq])
                dma("act", y[tt * 128:(tt + 1) * 128, :], yt[:], reads=[yt])
        B.barrier()

    nsem = B._n_sems
    import contextlib
    with contextlib.ExitStack() as es:
        sems = [es.enter_context(nc.semaphore(f"s{i}")) for i in range(nsem)]
        B.sem_handles = sems
        block = es.enter_context(nc.Block())

        @block.sync
        def _(q):
            B.emit("sp", q)

        @block.tensor
        def _(q):
            B.emit("pe", q)

        @block.scalar
        def _(q):
            B.emit("act", q)

        @block.vector
        def _(q):
            B.emit("dve", q)

        @block.gpsimd
        def _(q):
            B.emit("pool", q)
    return nc


_BF = ml_dtypes.bfloat16
_NC_CACHE = {}


def make_consts(S):
    NB = 16
    c = {}
    c["c_ident"] = np.eye(128, dtype=np.float32).astype(_BF)
    j = np.arange(128)[:, None]
    k = np.arange(128)[None, :]
    c["c_ustrict"] = (-(j > k).astype(np.float32)).astype(_BF)
    c["c_nlt"] = (-(j <= k).astype(np.float32)).astype(_BF)
    p = np.arange(128)[:, None, None]
    v = np.arange(4)[None, :, None]
    f = np.arange(512)[None, None, :]
    c["c_maskaddlt"] = np.where((v * 128 + p) < f, 0.0, -BIG).astype(np.float32)
    c["c_maskadd"] = np.where((v * 128 + p) <= f, 0.0, -BIG).astype(np.float32)
    own = np.arange(NB)[:, None, None]
    n = np.arange(NB)[None, None, :]
    past = np.broadcast_to(n < own, (NB, 8, NB)).reshape(NB, 128)
    isown = np.broadcast_to(n == own, (NB, 8, NB)).reshape(NB, 128)
    c["c_pastadd"] = np.ascontiguousarray(np.broadcast_to(np.where(past, 0.0, -1e30).astype(np.float32)[None], (128, NB, 128)))
    c["c_validpast"] = np.ascontiguousarray(np.broadcast_to(past.astype(np.float32)[None], (128, NB, 128))).astype(_BF)
    c["c_isown"] = np.ascontiguousarray(np.broadcast_to(isown.astype(np.float32)[None], (128, NB, 128))).astype(_BF)
    c["c_foxq"] = np.ones((8, 3, S), np.float32).astype(_BF)
    slopes = (2.0 ** (-8.0 * np.arange(1, 9) / 8)).astype(np.float32)[:, None]
    pos = np.arange(S)[None, :]
    blk = (pos // 256).astype(np.float32)
    off = (pos % 256).astype(np.float32)
    mq = np.zeros((8, 4, S), np.float32)
    mq[:, 0] = -slopes * 256.0 * blk
    mq[:, 1] = -slopes * off
    mq[:, 2] = 1.0
    mq[:, 3] = 1.0
    c["c_mobq"] = mq.astype(_BF)
    mk = np.zeros((8, 20, S), np.float32)
    for nb in range(NB):
        mk[:, nb] = (pos // 256 == nb).astype(np.float32)
    mk[:, 16] = 1.0
    mk[:, 17] = 1.0
    mk[:, 18] = slopes * 256.0 * blk
    mk[:, 19] = slopes * off
    c["c_mobk"] = mk.astype(_BF)
    return c


def layer_inputs(l0, l1, norm_mix, w_in, b_forget, w_branch, w_out, norm_mlp, w_up, w_down, norm_final):
    f32 = np.float32
    L = l1 - l0
    d = {}
    d["norm_mix"] = np.ascontiguousarray(np.asarray(norm_mix, f32)[l0:l1].reshape(L, 8, 128).transpose(0, 2, 1))
    d["norm_mlp"] = np.ascontiguousarray(np.asarray(norm_mlp, f32)[l0:l1].reshape(L, 8, 128).transpose(0, 2, 1))
    d["w_in"] = np.ascontiguousarray(np.asarray(w_in, f32)[l0:l1])
    d["b_forget"] = np.ascontiguousarray(np.asarray(b_forget, f32)[l0:l1].reshape(L, 8, 1))
    d["w_branch"] = np.ascontiguousarray(np.asarray(w_branch, f32)[l0:l1].reshape(L, 3 * BW, D))
    d["w_out"] = np.ascontiguousarray(np.asarray(w_out, f32)[l0:l1])
    d["w_up"] = np.ascontiguousarray(np.asarray(w_up, f32)[l0:l1])
    d["w_down"] = np.ascontiguousarray(np.asarray(w_down, f32)[l0:l1])
    d["norm_final"] = np.ascontiguousarray(np.broadcast_to(np.asarray(norm_final, f32)[None, :], (128, D)))
    return d


def kernel(x, norm_mix, w_in, b_forget, w_branch, w_out, norm_mlp, w_up, w_down, norm_final):
    x = np.asarray(x, np.float32)
    Bsz, S, _ = x.shape
    L = int(np.asarray(norm_mix).shape[0])
    key = (S, L)
    if key not in _NC_CACHE:
        _NC_CACHE[key] = build_program(S, L, first_layer=0, n_layers_total=L)
    nc = _NC_CACHE[key]
    base = layer_inputs(0, L, norm_mix, w_in, b_forget, w_branch, w_out, norm_mlp, w_up, w_down, norm_final)
    base.update(make_consts(S))
    n_cores = 8
    in_maps = []
    for c in range(n_cores):
        d = dict(base)
        d["x"] = np.ascontiguousarray(x[c % Bsz])
        in_maps.append(d)
    res = run_bass_kernel_spmd(nc, in_maps, core_ids=list(range(n_cores)))
    out = np.stack([np.asarray(res.results[b]["y"], np.float32) for b in range(Bsz)], axis=0)
    return out
```
